# Optimizing a Trainium2 kernel written in Bass

```python
import jax, jax.numpy as jnp
from jax import lax
import numpy as np

D_MODEL = 1024
BATCH = 8
SEQ = 4096
DEPTH = 4

GLA_HEADS = 4
GLA_V = D_MODEL // 2
GLA_DV = GLA_V // GLA_HEADS
GLA_DK = GLA_DV // 2
GLA_QK = GLA_HEADS * GLA_DK
GATE_RANK = 16
GATE_NORMALIZER = 16.0
GLA_CHUNK = 64
POOL_WINDOWS = (2, 4, 8, 16)
POOL_WIDTH = D_MODEL // 4
POOL_GROUP_DIM = POOL_WIDTH // len(POOL_WINDOWS)
LRU_WIDTH = D_MODEL // 4
LRU_BLOCKS = 4
LRU_BLOCK_DIM = LRU_WIDTH // LRU_BLOCKS
LRU_CONV = 4
LRU_C = 8.0
D_MIX = GLA_V + POOL_WIDTH + LRU_WIDTH
IN_SPLITS = (GLA_QK, GLA_QK, GLA_V, GLA_V, GATE_RANK, POOL_WIDTH, LRU_WIDTH, LRU_WIDTH)
N_IN = 2 * GLA_QK + 2 * GLA_V + GATE_RANK + POOL_WIDTH + 2 * LRU_WIDTH
FFN_DIM = 3 * D_MODEL
FFN_CONV = 3
EPS = 1e-6

kernel_name = 'hybrid_gla_pool_rglru_block'


def rms_norm(x, g):
    xf = x.astype(jnp.float32)
    y = xf * lax.rsqrt(jnp.mean(xf * xf, axis=-1, keepdims=True) + EPS)
    return (y * g.astype(jnp.float32)).astype(x.dtype)


def causal_dwconv(x, w, b):
    K = w.shape[0]
    S = x.shape[1]
    xp = jnp.pad(x, ((0, 0), (K - 1, 0), (0, 0)))
    y = b
    for k in range(K):
        y = y + xp[:, k:k + S] * w[k]
    return y


def gla_chunked(q, k, v, log_alpha):
    B, S, _ = q.shape
    n_chunks = S // GLA_CHUNK

    def to_chunks(t, d):
        t = t.astype(jnp.float32).reshape(B, n_chunks, GLA_CHUNK, GLA_HEADS, d)
        return t.transpose(1, 0, 3, 2, 4)

    qc = to_chunks(q, GLA_DK) * (GLA_DK ** -0.5)
    kc = to_chunks(k, GLA_DK)
    vc = to_chunks(v, GLA_DV)
    bc = jnp.cumsum(to_chunks(log_alpha, GLA_DK), axis=3)
    causal = jnp.tril(jnp.ones((GLA_CHUNK, GLA_CHUNK), dtype=bool))[:, :, None]

    def step(state, inp):
        q_c, k_c, v_c, b_c = inp
        o_inter = jnp.einsum('bhcd,bhde->bhce', q_c * jnp.exp(b_c), state)
        diff = b_c[:, :, :, None, :] - b_c[:, :, None, :, :]
        decay = jnp.exp(jnp.where(causal, diff, -jnp.inf))
        scores = jnp.sum(q_c[:, :, :, None, :] * k_c[:, :, None, :, :] * decay, axis=-1)
        o_intra = jnp.einsum('bhij,bhje->bhie', scores, v_c)
        b_last = b_c[:, :, -1:, :]
        k_dec = k_c * jnp.exp(b_last - b_c)
        state = jnp.exp(b_last[:, :, 0, :, None]) * state + jnp.einsum('bhcd,bhce->bhde', k_dec, v_c)
        return state, o_inter + o_intra

    state0 = jnp.zeros((B, GLA_HEADS, GLA_DK, GLA_DV), jnp.float32)
    _, o = lax.scan(step, state0, (qc, kc, vc, bc))
    return o.transpose(1, 0, 3, 2, 4).reshape(B, S, GLA_HEADS, GLA_DV)


def pool_mixer(u, w, scale):
    B, S, _ = u.shape
    uf = u.astype(jnp.float32)
    c = jnp.cumsum(uf, axis=1)
    pos = jnp.arange(1, S + 1, dtype=jnp.float32)[:, None]
    outs = []
    for gi, win in enumerate(POOL_WINDOWS):
        sl = slice(gi * POOL_GROUP_DIM, (gi + 1) * POOL_GROUP_DIM)
        cg = c[..., sl]
        prev = jnp.pad(cg, ((0, 0), (win, 0), (0, 0)))[:, :S]
        mean = (cg - prev) / jnp.minimum(pos, float(win))
        d = (mean - uf[..., sl]).astype(u.dtype)
        outs.append(jnp.einsum('bsi,ij->bsj', d, w[gi]))
    return jnp.concatenate(outs, axis=-1) * scale


def rg_lru(xc, w_a, b_a, w_x, b_x, lam):
    B, S, _ = xc.shape
    xb = xc.reshape(B, S, LRU_BLOCKS, LRU_BLOCK_DIM)
    r = jax.nn.sigmoid(jnp.einsum('bshi,hij->bshj', xb, w_a).reshape(B, S, LRU_WIDTH) + b_a)
    i = jax.nn.sigmoid(jnp.einsum('bshi,hij->bshj', xb, w_x).reshape(B, S, LRU_WIDTH) + b_x)
    log_a = -LRU_C * r.astype(jnp.float32) * jax.nn.softplus(-lam.astype(jnp.float32))
    a = jnp.exp(log_a)
    u = jnp.sqrt(-jnp.expm1(2.0 * log_a)) * (i * xc).astype(jnp.float32)

    def combine(left, right):
        a1, b1 = left
        a2, b2 = right
        return a1 * a2, a2 * b1 + b2

    _, h = lax.associative_scan(combine, (a, u), axis=1)
    return h.astype(xc.dtype)


def setup_inputs(seed: int = 0) -> dict:
    key = jax.random.key(seed)
    ks = jax.random.split(key, 24)

    def nrm(k, shape, fan_in):
        return jax.random.normal(k, shape, jnp.float32) * (fan_in ** -0.5)

    def gain(k, shape):
        return 1.0 + 0.02 * jax.random.normal(k, shape, jnp.float32)

    def bias(k, shape):
        return 0.02 * jax.random.normal(k, shape, jnp.float32)

    a0 = jax.random.uniform(ks[14], (DEPTH, LRU_WIDTH), jnp.float32, minval=0.9, maxval=0.999)
    s = a0 ** (1.0 / LRU_C)
    lam = jnp.log(s) - jnp.log1p(-s)
    return {
        'x': jax.random.normal(ks[0], (BATCH, SEQ, D_MODEL), jnp.float32),
        'norm1_g': gain(ks[1], (DEPTH, D_MODEL)),
        'w_in': nrm(ks[2], (DEPTH, D_MODEL, N_IN), D_MODEL),
        'gla_wg2': nrm(ks[3], (DEPTH, GATE_RANK, GLA_QK), GATE_RANK),
        'gla_bg': bias(ks[4], (DEPTH, GLA_QK)),
        'gla_norm_g': gain(ks[5], (DEPTH, GLA_HEADS, GLA_DV)),
        'pool_w': nrm(ks[6], (DEPTH, len(POOL_WINDOWS), POOL_GROUP_DIM, POOL_GROUP_DIM), POOL_GROUP_DIM),
        'pool_scale': gain(ks[7], (DEPTH, POOL_WIDTH)),
        'lru_conv_w': nrm(ks[8], (DEPTH, LRU_CONV, LRU_WIDTH), LRU_CONV),
        'lru_conv_b': bias(ks[9], (DEPTH, LRU_WIDTH)),
        'lru_wa': nrm(ks[10], (DEPTH, LRU_BLOCKS, LRU_BLOCK_DIM, LRU_BLOCK_DIM), LRU_BLOCK_DIM),
        'lru_ba': bias(ks[11], (DEPTH, LRU_WIDTH)),
        'lru_wx': nrm(ks[12], (DEPTH, LRU_BLOCKS, LRU_BLOCK_DIM, LRU_BLOCK_DIM), LRU_BLOCK_DIM),
        'lru_bx': bias(ks[13], (DEPTH, LRU_WIDTH)),
        'lru_lambda': lam,
        'w_out': nrm(ks[15], (DEPTH, D_MIX, D_MODEL), D_MIX),
        'norm2_g': gain(ks[16], (DEPTH, D_MODEL)),
        'ffn_w_up': nrm(ks[17], (DEPTH, D_MODEL, 2 * FFN_DIM), D_MODEL),
        'ffn_conv_w': nrm(ks[18], (DEPTH, FFN_CONV, 2 * FFN_DIM), FFN_CONV),
        'ffn_conv_b': bias(ks[19], (DEPTH, 2 * FFN_DIM)),
        'ffn_w_down': nrm(ks[20], (DEPTH, FFN_DIM, D_MODEL), FFN_DIM),
        'final_g': gain(ks[21], (D_MODEL,)),
    }


def reference(x, norm1_g, w_in, gla_wg2, gla_bg, gla_norm_g, pool_w, pool_scale,
              lru_conv_w, lru_conv_b, lru_wa, lru_ba, lru_wx, lru_bx, lru_lambda,
              w_out, norm2_g, ffn_w_up, ffn_conv_w, ffn_conv_b, ffn_w_down, final_g):
    B, S, _ = x.shape
    split_points = [int(p) for p in np.cumsum(IN_SPLITS)[:-1]]
    for l in range(DEPTH):
        h = rms_norm(x, norm1_g[l])
        z = jnp.einsum('bsd,dn->bsn', h, w_in[l])
        q, k, v, g, g_low, pool_u, lru_x, lru_y = jnp.split(z, split_points, axis=-1)

        gate_logits = jnp.einsum('bsr,rk->bsk', g_low, gla_wg2[l]) + gla_bg[l]
        log_alpha = jax.nn.log_sigmoid(gate_logits.astype(jnp.float32)) / GATE_NORMALIZER
        o_gla = gla_chunked(q, k, v, log_alpha)
        o_gla = rms_norm(o_gla, gla_norm_g[l]).astype(x.dtype)
        o_gla = (o_gla * jax.nn.silu(g.reshape(B, S, GLA_HEADS, GLA_DV))).reshape(B, S, GLA_V)

        o_pool = pool_mixer(pool_u, pool_w[l], pool_scale[l])

        xc = causal_dwconv(lru_x, lru_conv_w[l], lru_conv_b[l])
        o_lru = rg_lru(xc, lru_wa[l], lru_ba[l], lru_wx[l], lru_bx[l], lru_lambda[l])
        o_lru = o_lru * jax.nn.gelu(lru_y, approximate=True)

        mix = jnp.concatenate([o_gla, o_pool, o_lru], axis=-1)
        x = x + jnp.einsum('bsm,md->bsd', mix, w_out[l])

        h2 = rms_norm(x, norm2_g[l])
        up = jnp.einsum('bsd,df->bsf', h2, ffn_w_up[l])
        up = causal_dwconv(up, ffn_conv_w[l], ffn_conv_b[l])
        gate, val = jnp.split(up, 2, axis=-1)
        x = x + jnp.einsum('bsf,fd->bsd', jax.nn.gelu(gate, approximate=True) * val, ffn_w_down[l])
    return rms_norm(x, final_g)
```

```python
import numpy as np
from contextlib import ExitStack
import concourse.bass as bass
import concourse.mybir as mybir
from concourse.bass_utils import run_bass_kernel_spmd

F32 = mybir.dt.float32
BF16 = mybir.dt.bfloat16
AF = mybir.ActivationFunctionType
ALU = mybir.AluOpType

D = 1024
NT_TOK = 512
EPS = 1e-6
N_IN_SLABS, N_OUT_SLABS, N_UP_SLABS, N_DN_SLABS = 5, 2, 12, 8
SLAB_W = [4096] * (N_IN_SLABS + N_OUT_SLABS + N_UP_SLABS) + [3072] * N_DN_SLABS
SLAB_OFF = [0]
for _w in SLAB_W:
    SLAB_OFF.append(SLAB_OFF[-1] + _w)
WCOLS = SLAB_OFF[-1]
NSLAB = len(SLAB_W)
S_IN, S_OUT, S_UP, S_DN = 0, N_IN_SLABS, N_IN_SLABS + N_OUT_SLABS, N_IN_SLABS + N_OUT_SLABS + N_UP_SLABS

P_G1, P_G2, P_BG, P_GN, P_PSC, P_LCW, P_LCB, P_LBA, P_LBX, P_LAM, P_FCW, P_FCB = (
    0, 8, 16, 18, 22, 24, 32, 34, 36, 38, 40, 184)
P_LAYER = 232
C_MASK, C_RESET, C_INV = 0, 128, 640
NCN = 704

ENGS = ("pe", "act", "dve", "pool", "sp")


class Buf:
    __slots__ = ("name", "w", "r", "track")

    def __init__(self, name, track=True):
        self.name = name
        self.w = None
        self.r = {}
        self.track = track


class Sched:
    def __init__(self, nc):
        self.nc = nc
        self.ops = {e: [] for e in ENGS}
        self.dma_sems = {}
        self.dry = False

    def _add(self, eng, fn, reads, writes, dma_sem=None, nodeps=False):
        if self.dry:
            return None
        idx = len(self.ops[eng])
        keep = []
        if not nodeps:
            deps = []
            for b in reads:
                if b.w is not None:
                    deps.append((b.w, "raw"))
            for b in writes:
                if b.w is not None:
                    deps.append((b.w, "waw"))
                for tk in b.r.values():
                    deps.append((tk, "war"))
            for tk, kind in deps:
                if tk[0] == "E" and tk[1] == eng and dma_sem is None and eng == "pe":
                    continue
                keep.append(tk)
        if dma_sem is None:
            tok = ("E", eng, idx)
        else:
            ent = self.dma_sems.setdefault(dma_sem, [None, 0])
            ent[1] += 16
            tok = ("D", dma_sem, ent[1])
        for b in reads:
            if b.track:
                old = b.r.get(tok[1])
                if old is None or old[2] < tok[2]:
                    b.r[tok[1]] = tok
        for b in writes:
            b.w = tok
            b.r = {}
        self.ops[eng].append({"fn": fn, "deps": keep, "dma": dma_sem})
        return tok

    def pe(self, fn, reads=(), writes=()):
        return self._add("pe", fn, reads, writes)

    def act(self, fn, reads=(), writes=()):
        return self._add("act", fn, reads, writes)

    def dve(self, fn, reads=(), writes=()):
        return self._add("dve", fn, reads, writes)

    def pool(self, fn, reads=(), writes=()):
        return self._add("pool", fn, reads, writes)

    def dma(self, queue, fn, sem, reads=(), writes=(), nodeps=False):
        return self._add(queue, fn, reads, writes, dma_sem=sem, nodeps=nodeps)

    def emit(self, final_waits=()):
        nc = self.nc
        signaled = {e: set() for e in ENGS}
        for e in ENGS:
            for op in self.ops[e]:
                for tk in op["deps"]:
                    if tk[0] == "E":
                        signaled[tk[1]].add(tk[2])
        cum = {}
        for e in ENGS:
            c = 0
            m = {}
            for i in range(len(self.ops[e])):
                if i in signaled[e]:
                    c += 1
                    m[i] = c
            cum[e] = m
        with ExitStack() as es:
            esem = {e: es.enter_context(nc.semaphore("s_" + e)) for e in ENGS}
            for name, ent in self.dma_sems.items():
                ent[0] = es.enter_context(nc.semaphore("d_" + name))
            block = es.enter_context(nc.Block())

            def run(ename, eng):
                waited = {}
                for i, op in enumerate(self.ops[ename]):
                    need = {}
                    for tk in op["deps"]:
                        if tk[0] == "E":
                            key = ("E", tk[1])
                            val = cum[tk[1]][tk[2]]
                        else:
                            key = ("D", tk[1])
                            val = tk[2]
                        if need.get(key, 0) < val:
                            need[key] = val
                    for key, val in need.items():
                        if waited.get(key, 0) >= val:
                            continue
                        waited[key] = val
                        sem = esem[key[1]] if key[0] == "E" else self.dma_sems[key[1]][0]
                        eng.wait_ge(sem, val)
                    ins = op["fn"](eng)
                    if op["dma"] is not None:
                        ins.then_inc(self.dma_sems[op["dma"]][0], 16)
                    elif i in signaled[ename]:
                        ins.then_inc(esem[ename], 1)
                if ename == "act":
                    for name in final_waits:
                        ent = self.dma_sems[name]
                        eng.wait_ge(ent[0], ent[1])

            @block.sync
            def _(eng):
                run("sp", eng)

            @block.tensor
            def _(eng):
                run("pe", eng)

            @block.scalar
            def _(eng):
                run("act", eng)

            @block.vector
            def _(eng):
                run("dve", eng)

            @block.gpsimd
            def _(eng):
                run("pool", eng)


class Ring:
    def __init__(self, name, n, apf):
        self.bufs = [Buf(f"{name}{i}") for i in range(n)]
        self.apf = apf
        self.n = n
        self.i = 0

    def get(self):
        i = self.i
        self.i = (i + 1) % self.n
        return self.bufs[i], self.apf(i)


class Cx:
    pass


def build_nc(S_len, DEPTH):
    NT = S_len // NT_TOK
    assert NT >= 2 and NT % 2 == 0
    N = NT_TOK
    NPK = DEPTH * P_LAYER + 8
    nc = bass.Bass("TRN2", target_bir_lowering=False)
    xT = nc.dram_tensor("xT", [D, S_len], F32, kind="ExternalInput").ap()
    w32 = nc.dram_tensor("w32", [DEPTH, 128, WCOLS], F32, kind="ExternalInput").ap()
    smat = nc.dram_tensor("smat", [DEPTH, 128, 1024], F32, kind="ExternalInput").ap()
    pk = nc.dram_tensor("pk", [128, NPK], F32, kind="ExternalInput").ap()
    cn = nc.dram_tensor("cn", [128, NCN], F32, kind="ExternalInput").ap()
    outT = nc.dram_tensor("outT", [D, S_len], F32, kind="ExternalOutput").ap()
    w16 = nc.dram_tensor("w16", [DEPTH, 128, WCOLS], BF16, kind="Internal").ap()
    S = Sched(nc)

    with ExitStack() as es:
        def sb(name, shape, dt):
            return es.enter_context(nc.sbuf_tensor(name, shape, dt))

        def psum(name, shape, dt):
            return es.enter_context(nc.psum_tensor(name, shape, dt))

        XTs = [sb(f"XT{i}", [128, 8, N], F32) for i in range(2)]
        bXs = [[Buf(f"X{i}_{k}") for k in range(8)] for i in range(2)]
        MIX = sb("MIX", [128, 8, N], BF16)
        bMIX = [Buf(f"MIX{k}") for k in range(8)]
        HF = sb("HF", [128, 8, N], BF16)
        bHF = [Buf(f"HF{k}") for k in range(8)]
        A24 = sb("A24", [128, 24, N], BF16)
        bA24 = [Buf(f"A24_{j}") for j in range(24)]
        PK = sb("PK", [128, NPK], F32)
        bPK = Buf("PK", track=False)
        DPK = sb("DPK", [128, DEPTH, 4], F32)
        bDPK = Buf("DPK", track=False)
        SM = sb("SM", [128, DEPTH, 1024], BF16)
        bSM = Buf("SM", track=False)
        CN = sb("CN", [128, NCN], F32)
        bCN = Buf("CN", track=False)
        MASKB = sb("MASKB", [128, 128], BF16)
        ONES = sb("ONES", [128, 128], BF16)
        IDN = sb("IDN", [128, 128], BF16)
        IDF = sb("IDF", [128, 128], F32)
        bONES = Buf("ONES", track=False)
        bMASK = Buf("MASK", track=False)
        bIDN = Buf("IDN", track=False)
        bIDF = Buf("IDF")
        V_TM = sb("V_TM", [128, 4, N], BF16)
        bV = [Buf(f"V{b}") for b in range(4)]
        KD_TM = sb("KD_TM", [128, 2, N], BF16)
        bKD = [Buf(f"KD{c}") for c in range(2)]
        QE = sb("QE", [128, 2, N], BF16)
        bQE = [Buf(f"QE{c}") for c in range(2)]
        KEZ = sb("KEZ", [128, 4, N], BF16)
        bKE = [Buf(f"KE{c}") for c in range(2)]
        SILU = sb("SILU", [128, 4, N], BF16)
        bSILU = [Buf(f"SILU{h}") for h in range(4)]
        GY = sb("GY", [128, 2, N], BF16)
        bGY = [Buf(f"GY{c}") for c in range(2)]
        U = sb("U", [128, 2, 528], F32)
        bU = [Buf(f"U{c}") for c in range(2)]
        XR = sb("XR", [128, 2, 516], F32)
        bXR = [Buf(f"XR{c}") for c in range(2)]
        SCM = sb("SCM", [128, 2, N], BF16)
        scring = Ring("SCM", 2, lambda i: SCM[:, i, :])
        Dd = sb("Dd", [128, 2, 4], F32)
        bDd = [Buf(f"Dd{c}") for c in range(2)]
        Sf = sb("Sf", [128, DEPTH, 2, 128], F32)
        Sb = sb("Sb", [128, DEPTH, 4, 128], BF16)
        bSf = [[Buf(f"Sf{l}_{hp}") for hp in range(2)] for l in range(DEPTH)]
        bSb = [[Buf(f"Sb{l}_{hp}") for hp in range(2)] for l in range(DEPTH)]
        HL = sb("HL", [128, DEPTH, 2], F32)
        bHL = [[Buf(f"HL{l}_{c}") for c in range(2)] for l in range(DEPTH)]
        UH = sb("UH", [128, DEPTH, 2, 16], F32)
        bUH = [[Buf(f"UH{l}_{c}") for c in range(2)] for l in range(DEPTH)]
        XH = sb("XH", [128, DEPTH, 2, 4], F32)
        bXH = [[Buf(f"XH{l}_{c}") for c in range(2)] for l in range(DEPTH)]
        FH = sb("FH", [128, DEPTH, 48, 2], F32)
        bFH = [Buf(f"FH{l}") for l in range(DEPTH)]
        CORR = sb("CORR", [128, 48, 2], F32)
        bCORR = Buf("CORR")
        CTMP = sb("CTMP", [128, 48], F32)
        bCTMP = Buf("CTMP")
        NFM, NFF, NBM, NBF, NWM, NWF = 10, 5, 5, 4, 2, 3
        FRM = sb("FRM", [128, NFM, 528], F32)
        FRF = sb("FRF", [128, NFF, N], F32)
        BRM = sb("BRM", [128, NBM, N], BF16)
        BRF = sb("BRF", [128, NBF, N], BF16)
        WRM = sb("WRM", [128, NWM, 4096], BF16)
        WRF = sb("WRF", [128, NWF, 4096], BF16)
        PSB = [psum(f"PS{i}", [128, N], F32) for i in range(7)]
        PST = psum("PST", [128, 1024], BF16)
        bPST = Buf("PST")

        CM, CF = Cx(), Cx()
        CM.H, CM.bH = MIX, bMIX
        CM.fring = Ring("FM", NFM, lambda i: FRM[:, i, :])
        CM.bring = Ring("BM", NBM, lambda i: BRM[:, i, :])
        CM.pring = Ring("PM", 4, lambda i: PSB[i])
        CM.wring = Ring("WM", NWM, lambda i: i)
        CM.WR, CM.wname = WRM, "wm"
        CF.H, CF.bH = HF, bHF
        CF.fring = Ring("FF", NFF, lambda i: FRF[:, i, :])
        CF.bring = Ring("BF", NBF, lambda i: BRF[:, i, :])
        CF.pring = Ring("PF", 3, lambda i: PSB[4 + i])
        CF.wring = Ring("WF", NWF, lambda i: i)
        CF.WR, CF.wname = WRF, "wf"

        def ACT(out, in_, func, reads, writes, **kw):
            S.act(lambda e: e.activation(out=out, in_=in_, func=func, **kw), reads, writes)

        def MM(out, lhsT, rhs, start, stop, reads, writes):
            S.pe(lambda e: e.matmul(out, lhsT=lhsT, rhs=rhs, start=start, stop=stop), reads, writes)

        def STT(out, in0, scalar, in1, op0, op1, reads, writes):
            S.dve(lambda e: e.scalar_tensor_tensor(out=out, in0=in0, scalar=scalar, in1=in1, op0=op0, op1=op1),
                  reads, writes)

        def TT(eng, out, in0, in1, op, reads, writes):
            getattr(S, eng)(lambda e: e.tensor_tensor(out=out, in0=in0, in1=in1, op=op), reads, writes)

        def TS(eng, out, in0, s1, s2, op0, op1, reads, writes):
            if s2 is None:
                getattr(S, eng)(lambda e: e.tensor_scalar(out=out, in0=in0, scalar1=s1, scalar2=None, op0=op0),
                                reads, writes)
            else:
                getattr(S, eng)(lambda e: e.tensor_scalar(out=out, in0=in0, scalar1=s1, scalar2=s2, op0=op0, op1=op1),
                                reads, writes)

        def CP(eng, out, in_, reads, writes):
            if eng == "act":
                S.act(lambda e: e.activation(out=out, in_=in_, func=AF.Copy), reads, writes)
            else:
                getattr(S, eng)(lambda e: e.tensor_copy(out=out, in_=in_), reads, writes)

        def pcol(l, off, n=1):
            base = l * P_LAYER + off
            return PK[:, base:base + n]

        S.dma("sp", lambda e: e.dma_start(out=PK[:, :], in_=pk[:, :]), "pk", writes=[bPK])
        S.dma("sp", lambda e: e.dma_start(out=CN[:, :], in_=cn[:, :]), "cn", writes=[bCN])

        def load_x(t):
            ts_ = slice(t * N, (t + 1) * N)
            S.dma("sp", lambda e: e.dma_start(
                out=XTs[t % 2][:, :, :], in_=xT[:, ts_].rearrange("(k p) s -> p k s", p=128)),
                f"xl{t % 2}", writes=bXs[t % 2])

        load_x(0)
        load_x(1)
        for l in range(DEPTH):
            S.dma("pool", (lambda l: lambda e: e.dma_start(out=SM[:, l, :], in_=smat[l, :, :]))(l), "sm",
                  writes=[bSM], nodeps=True)
        bW16 = [[Buf(f"W16A_{l}", track=False), Buf(f"W16B_{l}", track=False)] for l in range(DEPTH)]
        for l in range(DEPTH):
            for s in range(NSLAB):
                o, wd = SLAB_OFF[s], SLAB_W[s]
                bb = 2048 if wd == 4096 else 1536
                grp = 0 if s < S_UP else 1
                S.dma("pool", (lambda l, o, wd, bb: lambda e: e.dma_start(
                    out=w16[l, :, o:o + wd].rearrange("p (a b) -> p a b", b=bb),
                    in_=w32[l, :, o:o + wd].rearrange("p (a b) -> p a b", b=bb)))(l, o, wd, bb),
                    f"cv{l}_{grp}", writes=[bW16[l][grp]], nodeps=True)
        S.dve(lambda e: e.memset(ONES[:, :], 1.0), writes=[bONES])
        S.dve(lambda e: e.tensor_copy(out=MASKB[:, :], in_=CN[:, C_MASK:C_MASK + 128]), reads=[bCN], writes=[bMASK])
        S.dve(lambda e: e.memset(IDF[:, :], 1.0), writes=[bIDF])
        S.pool(lambda e: e.affine_select(out=IDF[:, :], in_=IDF[:, :], pattern=[[1, 128]], base=0,
                                         channel_multiplier=-1, compare_op=ALU.is_equal, fill=0.0),
               reads=[bIDF], writes=[bIDF])
        S.pool(lambda e: e.tensor_copy(out=IDN[:, :], in_=IDF[:, :]), reads=[bIDF], writes=[bIDN])
        S.pool(lambda e: e.memset(KEZ[:, :, :], 0.0), writes=bKE)
        for (tl, bl) in ((Sf, bSf), (Sb, bSb)):
            S.pool((lambda tl: lambda e: e.memset(tl[:, :, :, :], 0.0))(tl), writes=[b for r in bl for b in r])
        S.pool(lambda e: e.memset(HL[:, :, :], 0.0), writes=[b for r in bHL for b in r])
        S.pool(lambda e: e.memset(UH[:, :, :, :], 0.0), writes=[b for r in bUH for b in r])
        S.pool(lambda e: e.memset(XH[:, :, :, :], 0.0), writes=[b for r in bXH for b in r])
        S.pool(lambda e: e.memset(FH[:, :, :, :], 0.0), writes=bFH)
        for l in range(DEPTH):
            TS("dve", DPK[:, l, 0:2], pcol(l, P_BG, 2), -1.0, None, ALU.mult, None, [bPK], [bDPK])
            fb, ft = CM.fring.get()
            ACT(ft[:, 0:2], pcol(l, P_LAM, 2), AF.Exp, [bPK], [fb], scale=-1.0)
            ACT(ft[:, 2:4], ft[:, 0:2], AF.Ln, [fb], [fb], bias=1.0)
            TS("dve", DPK[:, l, 2:4], ft[:, 2:4], -8.0, None, ALU.mult, None, [fb], [bDPK])

        def load_slab(cx, l, s):
            b, slot = cx.wring.get()
            o, wd = SLAB_OFF[s], SLAB_W[s]
            grp = 0 if s < S_UP else 1
            WRt = cx.WR
            S.dma("sp", lambda e: e.dma_start(out=WRt[:, slot, 0:wd], in_=w16[l, :, o:o + wd]),
                  f"{cx.wname}{slot}", reads=[bW16[l][grp]], writes=[b])
            return b, slot

        def rmsnorm(cx, X, bX):
            pb, pt = cx.pring.get()
            for k in range(8):
                qb, qt = cx.bring.get()
                ACT(qt, X[:, k, :], AF.Square, [bX[k]], [qb])
                MM(pt[:, :], ONES[:, :], qt, k == 0, k == 7, [bONES, qb], [pb])
            lb, lt = cx.fring.get()
            ACT(lt[:, 0:N], pt[:, :], AF.Ln, [pb], [lb], scale=1.0 / D, bias=EPS)
            rb, rt = cx.fring.get()
            ACT(rt[:, 0:N], lt[:, 0:N], AF.Exp, [lb], [rb], scale=-0.5)
            return rb, rt

        def norm_to_H(cx, X, bX, gcol_base):
            rb, rt = rmsnorm(cx, X, bX)
            for k in range(8):
                STT(cx.H[:, k, :], X[:, k, :], PK[:, gcol_base + k:gcol_base + k + 1], rt[:, 0:N],
                    ALU.mult, ALU.mult, [bX[k], bPK, rb], [cx.bH[k]])

        def norm_gen(cx, X, bX, gcol_base, ph):
            pb, pt = cx.pring.get()
            for k in range(8):
                qb, qt = cx.bring.get()
                ACT(qt, X[:, k, :], AF.Square, [bX[k]], [qb])
                MM(pt[:, :], ONES[:, :], qt, k == 0, k == 7, [bONES, qb], [pb])
                if k == 3:
                    yield ph
            yield ph
            lb, lt = cx.fring.get()
            ACT(lt[:, 0:N], pt[:, :], AF.Ln, [pb], [lb], scale=1.0 / D, bias=EPS)
            ACT(lt[:, 0:N], lt[:, 0:N], AF.Exp, [lb], [lb], scale=-0.5)
            yield ph
            for k in range(8):
                STT(cx.H[:, k, :], X[:, k, :], PK[:, gcol_base + k:gcol_base + k + 1], lt[:, 0:N],
                    ALU.mult, ALU.mult, [bX[k], bPK, lb], [cx.bH[k]])
                if k % 2 == 1:
                    yield ph

        def fm_group(cx, slot, wb, col0, M=128):
            pb, pt = cx.pring.get()
            for k in range(8):
                MM(pt[0:M, :], cx.WR[:, slot, k * 512 + col0:k * 512 + col0 + M], cx.H[:, k, :], k == 0, k == 7,
                   [wb, cx.bH[k]], [pb])
            return pb, pt

        def mixer_gen(t, l):
            cx = CM
            X, bX = XTs[t % 2], bXs[t % 2]
            H, bH = cx.H, cx.bH
            fring, bring, pring = cx.fring, cx.bring, cx.pring
            first_tile = (t == 0)
            yield from norm_gen(cx, X, bX, l * P_LAYER + P_G1, 1)
            wb, slot = load_slab(cx, l, S_IN + 0)
            pb, pt = fm_group(cx, slot, wb, 256, M=16)
            gb_, GLOW = bring.get()
            ACT(GLOW[0:16, :], pt[0:16, :], AF.Copy, [pb], [gb_])
            yield 1
            cs = []
            for c in range(2):
                pb, pt = pring.get()
                MM(pt[:, :], SM[0:16, l, c * 128:(c + 1) * 128], GLOW[0:16, :], True, True, [bSM, gb_], [pb])
                eb_, et_ = fring.get()
                ACT(et_[:, 0:N], pt[:, :], AF.Exp, [pb, bDPK], [eb_], scale=-1.0, bias=DPK[:, l, c:c + 1])
                sb_, st_ = fring.get()
                ACT(st_[:, 0:N], et_[:, 0:N], AF.Ln, [eb_], [sb_], bias=1.0)
                cb_, ct_ = fring.get()
                S.dve((lambda ct_, st_: lambda e: e.tensor_tensor_scan(
                    out=ct_[:, 0:N], data0=CN[:, C_RESET:C_RESET + N], data1=st_[:, 0:N], initial=0.0,
                    op0=ALU.mult, op1=ALU.add))(ct_, st_), [sb_, bCN], [cb_])
                cs.append((cb_, ct_))
                yield 1
            ebs, enbs = [], []
            for c in range(2):
                cb_, ct_ = cs[c]
                b1, t1 = fring.get()
                ACT(t1[:, 0:N], ct_[:, 0:N], AF.Exp, [cb_], [b1], scale=-1.0 / 16)
                b2, t2 = fring.get()
                ACT(t2[:, 0:N], ct_[:, 0:N], AF.Exp, [cb_], [b2], scale=1.0 / 16)
                ACT(Dd[:, c, :], ct_[:, 127:N:128], AF.Exp, [cb_], [bDd[c]], scale=-1.0 / 16)
                ebs.append((b1, t1))
                enbs.append((b2, t2))
            for c in range(2):
                pb, pt = fm_group(cx, slot, wb, c * 128)
                ACT(GY[:, c, :], pt[:, :], AF.Gelu_apprx_tanh, [pb], [bGY[c]])
                yield 1
            wb, slot = load_slab(cx, l, S_IN + 1)
            for c in range(2):
                pb, pt = fm_group(cx, slot, wb, c * 128)
                STT(QE[:, c, :], pt[:, :], 0.125, ebs[c][1][:, 0:N], ALU.mult, ALU.mult, [pb, ebs[c][0]], [bQE[c]])
                yield 1
            for c in range(2):
                pb, pt = fm_group(cx, slot, wb, 256 + c * 128)
                ent = enbs[c][1]
                for a in range(2):
                    ps_ = slice(a * 64, (a + 1) * 64)
                    TT("dve", KEZ[ps_, 2 * c + a, :], pt[ps_, :], ent[ps_, 0:N], ALU.mult, [pb, enbs[c][0]], [bKE[c]])
                db_, dt_ = fring.get()
                for blk in range(4):
                    TS("dve", dt_[:, blk * 128:(blk + 1) * 128], ent[:, blk * 128:(blk + 1) * 128],
                       Dd[:, c, blk:blk + 1], None, ALU.mult, None, [enbs[c][0], bDd[c]], [db_])
                kb_, KDT = bring.get()
                TT("dve", KDT, pt[:, :], dt_[:, 0:N], ALU.mult, [pb, db_], [kb_])
                yield 1
                for blk in range(4):
                    S.pe((lambda KDT, blk: lambda e: e.transpose(
                        PST[:, blk * 128:(blk + 1) * 128], KDT[:, blk * 128:(blk + 1) * 128], IDN[:, :]))(KDT, blk),
                        [kb_, bIDN], [bPST])
                CP("act", KD_TM[:, c, :], PST[:, 0:N], [bPST], [bKD[c]])
                yield 1
            wb, slot = load_slab(cx, l, S_IN + 2)
            for blk in range(4):
                pb, pt = pring.get()
                for k in range(8):
                    MM(pt[:, :], H[:, k, blk * 128:(blk + 1) * 128], cx.WR[:, slot, k * 512:(k + 1) * 512],
                       k == 0, k == 7, [wb, bH[k]], [pb])
                CP("act", V_TM[:, blk, :], pt[:, :], [pb], [bV[blk]])
                yield 1
            wb, slot = load_slab(cx, l, S_IN + 3)
            for h in range(4):
                pb, pt = fm_group(cx, slot, wb, h * 128)
                fb, ft = fring.get()
                ACT(ft[:, 0:N], pt[:, :], AF.Silu, [pb], [fb])
                TS("dve", SILU[:, h, :], ft[:, 0:N], pcol(l, P_GN + h), None, ALU.mult, None, [fb, bPK], [bSILU[h]])
                yield 1
            wb, slot = load_slab(cx, l, S_IN + 4)
            for c in range(2):
                CP("pool", U[:, c, 0:16], UH[:, l, c, :], [bUH[l][c]], [bU[c]])
                pb, pt = fm_group(cx, slot, wb, c * 128)
                CP("act", U[:, c, 16:528], pt[:, :], [pb], [bU[c]])
                CP("pool", UH[:, l, c, :], U[:, c, 512:528], [bU[c]], [bUH[l][c]])
                yield 1
            for c in range(2):
                CP("pool", XR[:, c, 0:3], XH[:, l, c, 0:3], [bXH[l][c]], [bXR[c]])
                pb, pt = fm_group(cx, slot, wb, 256 + c * 128)
                CP("act", XR[:, c, 3:515], pt[:, :], [pb], [bXR[c]])
                CP("pool", XH[:, l, c, 0:3], XR[:, c, 512:515], [bXR[c]], [bXH[l][c]])
                yield 1

            psrc = []
            for c in range(2):
                u = U[:, c, :]
                b2, t2 = fring.get()
                TT("pool", t2[:, 1:528], u[:, 1:528], u[:, 0:527], ALU.add, [bU[c]], [b2])
                b4, t4 = fring.get()
                TT("pool", t4[:, 3:528], t2[:, 3:528], t2[:, 1:526], ALU.add, [b2], [b4])
                if c == 0:
                    psrc.append(((b2, t2, 2, 0), (b4, t4, 4, 1)))
                else:
                    b8, t8 = fring.get()
                    TT("pool", t8[:, 7:528], t4[:, 7:528], t4[:, 3:524], ALU.add, [b4], [b8])
                    b16, t16 = fring.get()
                    TT("pool", t16[:, 15:528], t8[:, 15:528], t8[:, 7:520], ALU.add, [b8], [b16])
                    psrc.append(((b8, t8, 8, 2), (b16, t16, 16, 3)))
            xcs = []
            for c in range(2):
                xb_, xc = fring.get()
                TS("pool", xc[:, 0:N], XR[:, c, 3:515], pcol(l, P_LCW + c * 4 + 3), pcol(l, P_LCB + c),
                   ALU.mult, ALU.add, [bXR[c], bPK], [xb_])
                xcs.append((xb_, xc))
            yield 2
            yield 2
            yield 2
            dpls = []
            if first_tile:
                fb, ft = fring.get()
            for c in range(2):
                u = U[:, c, :]
                dpb, DPLc = bring.get()
                for a, (sbuf_, stile, win, wi) in enumerate(psrc[c]):
                    ps_ = slice(a * 64, (a + 1) * 64)
                    STT(DPLc[ps_, :], stile[ps_, 16:528], 1.0 / win, u[ps_, 16:528], ALU.mult, ALU.subtract,
                        [sbuf_, bU[c]], [dpb])
                    if first_tile:
                        fcol = slice(c * 16, (c + 1) * 16)
                        TT("dve", ft[ps_, fcol], stile[ps_, 16:32], CN[ps_, C_INV + wi * 16:C_INV + (wi + 1) * 16],
                           ALU.mult, [sbuf_, bCN], [fb])
                        TT("dve", DPLc[ps_, 0:16], ft[ps_, fcol], u[ps_, 16:32], ALU.subtract,
                           [fb, bU[c], dpb], [dpb])
                dpls.append((dpb, DPLc))
            for c in range(2):
                xb_, xc = xcs[c]
                for k in range(3):
                    STT(xc[:, 0:N], XR[:, c, k:k + N], pcol(l, P_LCW + c * 4 + k), xc[:, 0:N], ALU.mult, ALU.add,
                        [bXR[c], bPK, xb_], [xb_])
            yield 2
            yield 2
            cbs = []
            for c in range(2):
                cbb, cbt = bring.get()
                CP("pool", cbt, xcs[c][1][:, 0:N], [xcs[c][0]], [cbb])
                cbs.append((cbb, cbt))
            pps = []
            for c in range(2):
                pb, pt = pring.get()
                MM(pt[:, :], SM[:, l, 256 + c * 128:256 + (c + 1) * 128], dpls[c][1], True, True, [bSM, dpls[c][0]], [pb])
                pps.append((pb, pt))
            yield 2
            yield 2
            for c in range(2):
                pb, pt = pps[c]
                ACT(MIX[:, 4 + c, :], pt[:, :], AF.Identity, [pb, bPK], [bMIX[4 + c]], scale=pcol(l, P_PSC + c))
            lr = []
            for c in range(2):
                cbb, cbt = cbs[c]
                pa, pat = pring.get()
                MM(pat[:, :], SM[:, l, 512 + c * 128:512 + (c + 1) * 128], cbt, True, True, [bSM, cbb], [pa])
                pi, pit = pring.get()
                MM(pit[:, :], SM[:, l, 768 + c * 128:768 + (c + 1) * 128], cbt, True, True, [bSM, cbb], [pi])
                yield 2
                rb_, rt_ = fring.get()
                ACT(rt_[:, 0:N], pat[:, :], AF.Sigmoid, [pa, bPK], [rb_], bias=pcol(l, P_LBA + c))
                ib_, it_ = fring.get()
                ACT(it_[:, 0:N], pit[:, :], AF.Sigmoid, [pi, bPK], [ib_], bias=pcol(l, P_LBX + c))
                lr.append((rb_, rt_, ib_, it_))
            yield 2
            for c in range(2):
                rb_, rt_, ib_, it_ = lr[c]
                ACT(rt_[:, 0:N], rt_[:, 0:N], AF.Exp, [rb_, bDPK], [rb_], scale=DPK[:, l, 2 + c:3 + c])
                TT("pool", it_[:, 0:N], it_[:, 0:N], xcs[c][1][:, 0:N], ALU.mult, [ib_, xcs[c][0]], [ib_])
            yield 2
            qs = []
            for c in range(2):
                rb_, rt_, ib_, it_ = lr[c]
                a2b, a2t = fring.get()
                TT("dve", a2t[:, 0:N], rt_[:, 0:N], rt_[:, 0:N], ALU.mult, [rb_], [a2b])
                qs.append((a2b, a2t))
            yield 2
            for c in range(2):
                a2b, a2t = qs[c]
                ACT(a2t[:, 0:N], a2t[:, 0:N], AF.Sqrt, [a2b], [a2b], scale=-1.0, bias=1.0)
            yield 2
            yield 2
            for c in range(2):
                rb_, rt_, ib_, it_ = lr[c]
                a2b, a2t = qs[c]
                xb_, xc = xcs[c]
                TT("dve", a2t[:, 0:N], a2t[:, 0:N], it_[:, 0:N], ALU.mult, [a2b, ib_], [a2b])
                S.dve((lambda xc, rt_, a2t, c: lambda e: e.tensor_tensor_scan(
                    out=xc[:, 0:N], data0=rt_[:, 0:N], data1=a2t[:, 0:N], initial=HL[:, l, c:c + 1],
                    op0=ALU.mult, op1=ALU.add))(xc, rt_, a2t, c), [rb_, a2b, bHL[l][c]], [xb_])
                CP("dve", HL[:, l, c:c + 1], xc[:, N - 1:N], [xb_], [bHL[l][c]])
                TT("dve", MIX[:, 6 + c, :], xc[:, 0:N], GY[:, c, :], ALU.mult, [xb_, bGY[c]], [bMIX[6 + c]])
            yield 2

            g_sc, g_kv, g_st, g_ot, g_q, g_pre, g_ss, g_rt = {}, {}, {}, {}, {}, {}, {}, {}

            def gla_stage(s, blk):
                bs = slice(blk * 128, (blk + 1) * 128)
                if s == 0:
                    pb, pt = pring.get()
                    for h in range(4):
                        hp, a = divmod(h, 2)
                        MM(pt[:, h * 128:(h + 1) * 128], KEZ[:, h, bs], QE[:, hp, bs], True, True,
                           [bKE[hp], bQE[hp]], [pb])
                    g_sc[blk] = (pb, pt)
                    kb, kt = pring.get()
                    for hp in range(2):
                        MM(kt[:, hp * 256:(hp + 1) * 256], KD_TM[:, hp, bs], V_TM[:, blk, hp * 256:(hp + 1) * 256],
                           True, True, [bKD[hp], bV[blk]], [kb])
                    g_kv[blk] = (kb, kt)
                elif s == 1:
                    pb, pt = g_sc[blk]
                    sb_, st_ = scring.get()
                    TT("dve", st_.rearrange("p (h n) -> p h n", h=4), pt[:, :].rearrange("p (h n) -> p h n", h=4),
                       MASKB[:, :].unsqueeze(1).to_broadcast([128, 4, 128]), ALU.mult, [pb, bMASK], [sb_])
                    g_st[blk] = (sb_, st_)
                    kb, kt = g_kv[blk]
                    for hp in range(2):
                        for a in range(2):
                            ps_ = slice(a * 64, (a + 1) * 64)
                            STT(Sf[ps_, l, hp, :], Sf[ps_, l, hp, :], Dd[ps_, hp, blk:blk + 1],
                                kt[ps_, hp * 256 + a * 128:hp * 256 + (a + 1) * 128], ALU.mult, ALU.add,
                                [bSf[l][hp], bDd[hp], kb], [bSf[l][hp]])
                elif s == 2:
                    sb_, st_ = g_st[blk]
                    ob, ot = pring.get()
                    for h in range(4):
                        hp, a = divmod(h, 2)
                        MM(ot[:, h * 128:(h + 1) * 128], V_TM[:, blk, h * 128:(h + 1) * 128],
                           st_[:, h * 128:(h + 1) * 128], True, False, [bV[blk], sb_], [ob])
                        MM(ot[:, h * 128:(h + 1) * 128], Sb[:, l, h, :], QE[:, hp, bs], False, True,
                           [bSb[l][hp], bQE[hp]], [ob])
                    g_ot[blk] = (ob, ot)
                    for hp in range(2):
                        for a in range(2):
                            ps_ = slice(a * 64, (a + 1) * 64)
                            CP("act", Sb[ps_, l, 2 * hp + a, :], Sf[ps_, l, hp, :], [bSf[l][hp]], [bSb[l][hp]])
                elif s == 3:
                    ob, ot = g_ot[blk]
                    qb, qt = bring.get()
                    ACT(qt, ot[:, :], AF.Square, [ob], [qb])
                    g_q[blk] = (qb, qt)
                    tb, tt = fring.get()
                    TT("dve", tt[:, 0:N].rearrange("p (h n) -> p h n", h=4),
                       ot[:, :].rearrange("p (h n) -> p h n", h=4), SILU[:, :, bs], ALU.mult, [ob] + bSILU, [tb])
                    g_pre[blk] = (tb, tt)
                elif s == 4:
                    qb, qt = g_q[blk]
                    nb_, nt_ = pring.get()
                    MM(nt_[:, :], ONES[:, :], qt, True, True, [bONES, qb], [nb_])
                    g_ss[blk] = (nb_, nt_)
                elif s == 5:
                    nb_, nt_ = g_ss[blk]
                    lb, lt = fring.get()
                    ACT(lt[:, 0:N], nt_[:, :], AF.Ln, [nb_], [lb], scale=1.0 / 128, bias=EPS)
                    ACT(lt[:, 0:N], lt[:, 0:N], AF.Exp, [lb], [lb], scale=-0.5)
                    g_rt[blk] = (lb, lt)
                elif s == 6:
                    tb, tt = g_pre[blk]
                    lb, lt = g_rt[blk]
                    TT("dve", MIX[:, 0:4, bs], tt[:, 0:N].rearrange("p (h n) -> p h n", h=4),
                       lt[:, 0:N].rearrange("p (h n) -> p h n", h=4), ALU.mult, [tb, lb], bMIX[0:4])

            for slot in range(2 * 3 + 7):
                for s in range(6, -1, -1):
                    if (slot - s) % 2 == 0 and 0 <= (slot - s) // 2 < 4:
                        gla_stage(s, (slot - s) // 2)
                yield 2

            slabs = [load_slab(cx, l, S_OUT + i) for i in range(2)]
            for n in range(8):
                wb, slot = slabs[n // 4]
                col0 = (n % 4) * 128
                pb, pt = pring.get()
                for k in range(8):
                    MM(pt[:, :], cx.WR[:, slot, k * 512 + col0:k * 512 + col0 + 128], MIX[:, k, :], k == 0, k == 7,
                       [wb, bMIX[k]], [pb])
                TT("dve", X[:, n, :], X[:, n, :], pt[:, :], ALU.add, [bX[n], pb], [bX[n]])
                yield 3

        def ffn_pre(t, l):
            cx = CF
            X, bX = XTs[t % 2], bXs[t % 2]
            yield from norm_gen(cx, X, bX, l * P_LAYER + P_G2, 4)
            fcw = PK[:, l * P_LAYER + P_FCW:l * P_LAYER + P_FCW + 144].rearrange("p (g k) -> p g k", k=3)
            TT("dve", CORR[:, :, 1], FH[:, l, :, 1], fcw[:, :, 0], ALU.mult, [bFH[l], bPK], [bCORR])
            TT("dve", CORR[:, :, 0], FH[:, l, :, 1], fcw[:, :, 1], ALU.mult, [bFH[l], bPK], [bCORR])
            TT("dve", CTMP[:, :], FH[:, l, :, 0], fcw[:, :, 0], ALU.mult, [bFH[l], bPK], [bCTMP])
            TT("dve", CORR[:, :, 0], CORR[:, :, 0], CTMP[:, :], ALU.add, [bCORR, bCTMP], [bCORR])
            yield 4

        def ffn_gen(t, l):
            cx = CF
            X, bX = XTs[t % 2], bXs[t % 2]
            fring, bring, pring = cx.fring, cx.bring, cx.pring

            def conv_group(slot, wb, col0, g):
                pb, pt = fm_group(cx, slot, wb, col0)
                ab_, at_ = fring.get()
                base = l * P_LAYER + P_FCW + g * 3
                ACT(at_[:, 0:N], pt[:, :], AF.Identity, [pb, bPK], [ab_], scale=PK[:, base + 2:base + 3],
                    bias=pcol(l, P_FCB + g))
                STT(at_[:, 1:N], pt[:, 0:N - 1], PK[:, base + 1:base + 2], at_[:, 1:N], ALU.mult, ALU.add,
                    [pb, bPK, ab_], [ab_])
                STT(at_[:, 2:N], pt[:, 0:N - 2], PK[:, base:base + 1], at_[:, 2:N], ALU.mult, ALU.add,
                    [pb, bPK, ab_], [ab_])
                TT("dve", at_[:, 0:2], at_[:, 0:2], CORR[:, g, :], ALU.add, [ab_, bCORR], [ab_])
                CP("act", FH[:, l, g, :], pt[:, N - 2:N], [pb], [bFH[l]])
                return ab_, at_

            def gating(fch, gb, gt, vb, vt):
                ggb, ggt = bring.get()
                ACT(ggt, gt[:, 0:N], AF.Gelu_apprx_tanh, [gb], [ggb])
                TT("pool", A24[:, fch, :], ggt, vt[:, 0:N], ALU.mult, [ggb, vb], [bA24[fch]])

            pend = None
            for i in range(6):
                wg, sg = load_slab(cx, l, S_UP + 2 * i)
                wv, sv = load_slab(cx, l, S_UP + 2 * i + 1)
                for j in range(4):
                    fch = 4 * i + j
                    gb, gt = conv_group(sg, wg, j * 128, fch)
                    yield 8
                    vb, vt = conv_group(sv, wv, j * 128, 24 + fch)
                    if pend is not None:
                        gating(*pend)
                    pend = (fch, gb, gt, vb, vt)
                    yield 8
            gating(*pend)
            for n in range(8):
                wb, slot = load_slab(cx, l, S_DN + n)
                pb, pt = pring.get()
                for j in range(24):
                    MM(pt[:, :], cx.WR[:, slot, j * 128:(j + 1) * 128], A24[:, j, :], j == 0, j == 23,
                       [wb, bA24[j]], [pb])
                    if j % 8 == 7:
                        yield 8
                TT("dve", X[:, n, :], X[:, n, :], pt[:, :], ALU.add, [bX[n], pb], [bX[n]])
            if l == DEPTH - 1:
                rb, rt = rmsnorm(cx, X, bX)
                gb = DEPTH * P_LAYER
                for k in range(8):
                    STT(X[:, k, :], X[:, k, :], PK[:, gb + k:gb + k + 1], rt[:, 0:N], ALU.mult, ALU.mult,
                        [bX[k], bPK, rb], [bX[k]])
                ts_ = slice(t * N, (t + 1) * N)
                S.dma("act", lambda e: e.dma_start(
                    out=outT[:, ts_].rearrange("(k p) s -> p k s", p=128), in_=X[:, :, :]), f"st{t % 2}", reads=bX)
                if t + 2 < NT:
                    load_x(t + 2)
                yield 8

        TL = [(t, l) for pair in range(NT // 2) for l in range(DEPTH) for t in (2 * pair, 2 * pair + 1)]
        def mside(t, l):
            yield from mixer_gen(t, l)
            yield from ffn_pre(t, l)

        S.dry = True
        cnt = {1: 0, 2: 0, 3: 0, 4: 0}
        for ph in mside(2, 0):
            cnt[ph] += 1
        S.dry = False
        xs = [0]
        for ph in (1, 2, 3, 4):
            xs.append(xs[-1] + cnt[ph])
        ys = M_SCHED_Y

        def m_target(i):
            for j in range(1, len(xs)):
                if i <= xs[j]:
                    return ys[j - 1] + (ys[j] - ys[j - 1]) * (i - xs[j - 1]) / float(xs[j] - xs[j - 1])
            return ys[-1]

        for step in range(len(TL) + 1):
            gm = mside(*TL[step]) if step < len(TL) else None
            gf = ffn_gen(*TL[step - 1]) if step >= 1 else None
            pm = 0
            pf = 0.0
            while gm is not None or gf is not None:
                run_m = gm is not None and (gf is None or m_target(pm) <= pf / FFN_W)
                if run_m:
                    try:
                        next(gm)
                        pm += 1
                    except StopIteration:
                        gm = None
                else:
                    try:
                        pf += next(gf)
                    except StopIteration:
                        gf = None
        global _SBUF_LEFT
        _SBUF_LEFT = nc.sbuf_bytes_remaining
        S.emit(final_waits=["st0", "st1"])
    return nc


MIXER_W = 54.0
M_SCHED_Y = (0.02, 0.36, 0.80, 0.86, 0.95)
FFN_W = 8.0 * (48 + 24)


def _slab_k(Wc):
    return np.ascontiguousarray(Wc.reshape(8, 128, 512).transpose(1, 0, 2)).reshape(128, 4096)


def _fm(v, n):
    return np.ascontiguousarray(v.reshape(n, 128).T)


def prep_host(inp, DEPTH):
    w32 = np.zeros((DEPTH, 128, WCOLS), np.float32)
    smat = np.zeros((DEPTH, 128, 1024), np.float32)
    pk = np.zeros((128, DEPTH * P_LAYER + 8), np.float32)
    for l in range(DEPTH):
        wi = inp["w_in"][l]
        q, k, v, g = wi[:, 0:256], wi[:, 256:512], wi[:, 512:1024], wi[:, 1024:1536]
        glow, pu, lx, ly = wi[:, 1536:1552], wi[:, 1552:1808], wi[:, 1808:2064], wi[:, 2064:2320]
        z = np.zeros((1024, 240), np.float32)
        slabs = [np.concatenate([ly, glow, z], 1), np.concatenate([q, k], 1), v, g, np.concatenate([pu, lx], 1)]
        wo = inp["w_out"][l]
        slabs += [wo[:, 0:512], wo[:, 512:1024]]
        wu = inp["ffn_w_up"][l]
        for i in range(6):
            slabs += [wu[:, 512 * i:512 * (i + 1)], wu[:, 3072 + 512 * i:3072 + 512 * (i + 1)]]
        for s, sl in enumerate(slabs):
            w32[l, :, SLAB_OFF[s]:SLAB_OFF[s + 1]] = _slab_k(sl)
        wd = inp["ffn_w_down"][l]
        for n in range(8):
            blk = wd[:, n * 128:(n + 1) * 128].reshape(24, 128, 128).transpose(1, 0, 2).reshape(128, 3072)
            s = S_DN + n
            w32[l, :, SLAB_OFF[s]:SLAB_OFF[s + 1]] = blk
        smat[l, 0:16, 0:256] = inp["gla_wg2"][l]
        for c in range(2):
            for a in range(2):
                r = slice(a * 64, (a + 1) * 64)
                smat[l, r, 256 + c * 128 + a * 64:256 + c * 128 + (a + 1) * 64] = inp["pool_w"][l, 2 * c + a]
                smat[l, r, 512 + c * 128 + a * 64:512 + c * 128 + (a + 1) * 64] = inp["lru_wa"][l, 2 * c + a]
                smat[l, r, 768 + c * 128 + a * 64:768 + c * 128 + (a + 1) * 64] = inp["lru_wx"][l, 2 * c + a]
        b = l * P_LAYER
        pk[:, b + P_G1:b + P_G1 + 8] = _fm(inp["norm1_g"][l], 8)
        pk[:, b + P_G2:b + P_G2 + 8] = _fm(inp["norm2_g"][l], 8)
        pk[:, b + P_BG:b + P_BG + 2] = _fm(inp["gla_bg"][l], 2)
        pk[:, b + P_GN:b + P_GN + 4] = inp["gla_norm_g"][l].T
        pk[:, b + P_PSC:b + P_PSC + 2] = _fm(inp["pool_scale"][l], 2)
        for c in range(2):
            pk[:, b + P_LCW + c * 4:b + P_LCW + c * 4 + 4] = inp["lru_conv_w"][l][:, c * 128:(c + 1) * 128].T
        pk[:, b + P_LCB:b + P_LCB + 2] = _fm(inp["lru_conv_b"][l], 2)
        pk[:, b + P_LBA:b + P_LBA + 2] = _fm(inp["lru_ba"][l], 2)
        pk[:, b + P_LBX:b + P_LBX + 2] = _fm(inp["lru_bx"][l], 2)
        pk[:, b + P_LAM:b + P_LAM + 2] = _fm(inp["lru_lambda"][l], 2)
        fw = inp["ffn_conv_w"][l]
        pk[:, b + P_FCW:b + P_FCW + 144] = fw.reshape(3, 48, 128).transpose(2, 1, 0).reshape(128, 144)
        pk[:, b + P_FCB:b + P_FCB + 48] = _fm(inp["ffn_conv_b"][l], 48)
    pk[:, DEPTH * P_LAYER:DEPTH * P_LAYER + 8] = _fm(inp["final_g"], 8)
    cn = np.zeros((128, NCN), np.float32)
    j = np.arange(128)[:, None]
    i = np.arange(128)[None, :]
    cn[:, C_MASK:C_MASK + 128] = (j <= i)
    rs = np.ones(512, np.float32)
    rs[::128] = 0.0
    cn[:, C_RESET:C_RESET + 512] = rs[None, :]
    for wi_, win in enumerate((2, 4, 8, 16)):
        cn[:, C_INV + wi_ * 16:C_INV + (wi_ + 1) * 16] = (1.0 / np.minimum(np.arange(1, 17), win))[None, :]
    return w32, smat, pk, cn


_NC_CACHE = {}


def run(inp, S_len, DEPTH, n_cores=8):
    key = (S_len, DEPTH)
    if key not in _NC_CACHE:
        _NC_CACHE[key] = build_nc(S_len, DEPTH)
    nc = _NC_CACHE[key]
    w32, smat, pk, cn = prep_host(inp, DEPTH)
    x = inp["x"]
    in_maps = []
    for b in range(n_cores):
        in_maps.append({"xT": np.ascontiguousarray(x[b].T), "w32": w32, "smat": smat, "pk": pk, "cn": cn})
    res = run_bass_kernel_spmd(nc, in_maps, core_ids=list(range(n_cores)))
    out = np.stack([np.ascontiguousarray(res.results[b]["outT"].T) for b in range(n_cores)], 0)
    return out.astype(np.float32)


def kernel(**inputs):
    inp = {k: np.asarray(v) for k, v in inputs.items()}
    return run(inp, 4096, 4, 8)
```

```python
import numpy as np
from contextlib import ExitStack
import concourse.bass as bass
import concourse.mybir as mybir
from concourse.bass_utils import run_bass_kernel_spmd

F32 = mybir.dt.float32
BF16 = mybir.dt.bfloat16
AF = mybir.ActivationFunctionType
ALU = mybir.AluOpType

D = 1024
NT_TOK = 512
EPS = 1e-6
N_IN_SLABS, N_OUT_SLABS, N_UP_SLABS, N_DN_SLABS = 5, 2, 12, 8
SLAB_W = [4096] * (N_IN_SLABS + N_OUT_SLABS + N_UP_SLABS) + [3072] * N_DN_SLABS
SLAB_OFF = [0]
for _w in SLAB_W:
    SLAB_OFF.append(SLAB_OFF[-1] + _w)
WCOLS = SLAB_OFF[-1]
NSLAB = len(SLAB_W)
S_IN, S_OUT, S_UP, S_DN = 0, N_IN_SLABS, N_IN_SLABS + N_OUT_SLABS, N_IN_SLABS + N_OUT_SLABS + N_UP_SLABS

P_G1, P_G2, P_BG, P_GN, P_PSC, P_LCW, P_LCB, P_LBA, P_LBX, P_LAM, P_FCW, P_FCB = (
    0, 8, 16, 18, 22, 24, 32, 34, 36, 38, 40, 184)
P_LAYER = 232
C_MASK, C_RESET, C_INV = 0, 128, 640
NCN = 704

ENGS = ("pe", "act", "dve", "pool", "sp")


class Buf:
    __slots__ = ("name", "w", "r", "track")

    def __init__(self, name, track=True):
        self.name = name
        self.w = None
        self.r = {}
        self.track = track


class Sched:
    def __init__(self, nc):
        self.nc = nc
        self.ops = {e: [] for e in ENGS}
        self.dma_sems = {}
        self.dry = False

    def _add(self, eng, fn, reads, writes, dma_sem=None, nodeps=False):
        if self.dry:
            return None
        idx = len(self.ops[eng])
        keep = []
        if not nodeps:
            deps = []
            for b in reads:
                if b.w is not None:
                    deps.append((b.w, "raw"))
            for b in writes:
                if b.w is not None:
                    deps.append((b.w, "waw"))
                for tk in b.r.values():
                    deps.append((tk, "war"))
            for tk, kind in deps:
                if tk[0] == "E" and tk[1] == eng and dma_sem is None and eng == "pe":
                    continue
                keep.append(tk)
        if dma_sem is None:
            tok = ("E", eng, idx)
        else:
            ent = self.dma_sems.setdefault(dma_sem, [None, 0])
            ent[1] += 16
            tok = ("D", dma_sem, ent[1])
        for b in reads:
            if b.track:
                old = b.r.get(tok[1])
                if old is None or old[2] < tok[2]:
                    b.r[tok[1]] = tok
        for b in writes:
            b.w = tok
            b.r = {}
        self.ops[eng].append({"fn": fn, "deps": keep, "dma": dma_sem})
        return tok

    def pe(self, fn, reads=(), writes=()):
        return self._add("pe", fn, reads, writes)

    def act(self, fn, reads=(), writes=()):
        return self._add("act", fn, reads, writes)

    def dve(self, fn, reads=(), writes=()):
        return self._add("dve", fn, reads, writes)

    def pool(self, fn, reads=(), writes=()):
        return self._add("pool", fn, reads, writes)

    def dma(self, queue, fn, sem, reads=(), writes=(), nodeps=False):
        return self._add(queue, fn, reads, writes, dma_sem=sem, nodeps=nodeps)

    def emit(self, final_waits=()):
        nc = self.nc
        signaled = {e: set() for e in ENGS}
        for e in ENGS:
            for op in self.ops[e]:
                for tk in op["deps"]:
                    if tk[0] == "E":
                        signaled[tk[1]].add(tk[2])
        cum = {}
        for e in ENGS:
            c = 0
            m = {}
            for i in range(len(self.ops[e])):
                if i in signaled[e]:
                    c += 1
                    m[i] = c
            cum[e] = m
        with ExitStack() as es:
            esem = {e: es.enter_context(nc.semaphore("s_" + e)) for e in ENGS}
            for name, ent in self.dma_sems.items():
                ent[0] = es.enter_context(nc.semaphore("d_" + name))
            block = es.enter_context(nc.Block())

            def run(ename, eng):
                waited = {}
                for i, op in enumerate(self.ops[ename]):
                    need = {}
                    for tk in op["deps"]:
                        if tk[0] == "E":
                            key = ("E", tk[1])
                            val = cum[tk[1]][tk[2]]
                        else:
                            key = ("D", tk[1])
                            val = tk[2]
                        if need.get(key, 0) < val:
                            need[key] = val
                    for key, val in need.items():
                        if waited.get(key, 0) >= val:
                            continue
                        waited[key] = val
                        sem = esem[key[1]] if key[0] == "E" else self.dma_sems[key[1]][0]
                        eng.wait_ge(sem, val)
                    ins = op["fn"](eng)
                    if op["dma"] is not None:
                        ins.then_inc(self.dma_sems[op["dma"]][0], 16)
                    elif i in signaled[ename]:
                        ins.then_inc(esem[ename], 1)
                if ename == "act":
                    for name in final_waits:
                        ent = self.dma_sems[name]
                        eng.wait_ge(ent[0], ent[1])

            @block.sync
            def _(eng):
                run("sp", eng)

            @block.tensor
            def _(eng):
                run("pe", eng)

            @block.scalar
            def _(eng):
                run("act", eng)

            @block.vector
            def _(eng):
                run("dve", eng)

            @block.gpsimd
            def _(eng):
                run("pool", eng)


class Ring:
    def __init__(self, name, n, apf):
        self.bufs = [Buf(f"{name}{i}") for i in range(n)]
        self.apf = apf
        self.n = n
        self.i = 0

    def get(self):
        i = self.i
        self.i = (i + 1) % self.n
        return self.bufs[i], self.apf(i)


class Cx:
    pass


def build_nc(S_len, DEPTH):
    NT = S_len // NT_TOK
    assert NT >= 2 and NT % 2 == 0
    N = NT_TOK
    NPK = DEPTH * P_LAYER + 8
    nc = bass.Bass("TRN2", target_bir_lowering=False)
    xT = nc.dram_tensor("xT", [D, S_len], F32, kind="ExternalInput").ap()
    w32 = nc.dram_tensor("w32", [DEPTH, 128, WCOLS], F32, kind="ExternalInput").ap()
    smat = nc.dram_tensor("smat", [DEPTH, 128, 1024], F32, kind="ExternalInput").ap()
    pk = nc.dram_tensor("pk", [128, NPK], F32, kind="ExternalInput").ap()
    cn = nc.dram_tensor("cn", [128, NCN], F32, kind="ExternalInput").ap()
    outT = nc.dram_tensor("outT", [D, S_len], F32, kind="ExternalOutput").ap()
    w16 = nc.dram_tensor("w16", [DEPTH, 128, WCOLS], BF16, kind="Internal").ap()
    S = Sched(nc)

    with ExitStack() as es:
        def sb(name, shape, dt):
            return es.enter_context(nc.sbuf_tensor(name, shape, dt))

        def psum(name, shape, dt):
            return es.enter_context(nc.psum_tensor(name, shape, dt))

        XTs = [sb(f"XT{i}", [128, 8, N], F32) for i in range(2)]
        bXs = [[Buf(f"X{i}_{k}") for k in range(8)] for i in range(2)]
        MIX = sb("MIX", [128, 8, N], BF16)
        bMIX = [Buf(f"MIX{k}") for k in range(8)]
        HF = sb("HF", [128, 8, N], BF16)
        bHF = [Buf(f"HF{k}") for k in range(8)]
        A24 = sb("A24", [128, 24, N], BF16)
        bA24 = [Buf(f"A24_{j}") for j in range(24)]
        PK = sb("PK", [128, NPK], F32)
        bPK = Buf("PK", track=False)
        DPK = sb("DPK", [128, DEPTH, 4], F32)
        bDPK = Buf("DPK", track=False)
        SM = sb("SM", [128, DEPTH, 1024], BF16)
        bSM = Buf("SM", track=False)
        CN = sb("CN", [128, NCN], F32)
        bCN = Buf("CN", track=False)
        MASKB = sb("MASKB", [128, 128], BF16)
        ONES = sb("ONES", [128, 128], BF16)
        IDN = sb("IDN", [128, 128], BF16)
        IDF = sb("IDF", [128, 128], F32)
        bONES = Buf("ONES", track=False)
        bMASK = Buf("MASK", track=False)
        bIDN = Buf("IDN", track=False)
        bIDF = Buf("IDF")
        V_TM = sb("V_TM", [128, 4, N], BF16)
        bV = [Buf(f"V{b}") for b in range(4)]
        KD_TM = sb("KD_TM", [128, 2, N], BF16)
        bKD = [Buf(f"KD{c}") for c in range(2)]
        QE = sb("QE", [128, 2, N], BF16)
        bQE = [Buf(f"QE{c}") for c in range(2)]
        KEZ = sb("KEZ", [128, 4, N], BF16)
        bKE = [Buf(f"KE{c}") for c in range(2)]
        SILU = sb("SILU", [128, 4, N], BF16)
        bSILU = [Buf(f"SILU{h}") for h in range(4)]
        GY = sb("GY", [128, 2, N], BF16)
        bGY = [Buf(f"GY{c}") for c in range(2)]
        U = sb("U", [128, 2, 528], F32)
        bU = [Buf(f"U{c}") for c in range(2)]
        XR = sb("XR", [128, 2, 516], F32)
        bXR = [Buf(f"XR{c}") for c in range(2)]
        SCM = sb("SCM", [128, 2, N], BF16)
        scring = Ring("SCM", 2, lambda i: SCM[:, i, :])
        Dd = sb("Dd", [128, 2, 4], F32)
        bDd = [Buf(f"Dd{c}") for c in range(2)]
        Sf = sb("Sf", [128, DEPTH, 2, 128], F32)
        Sb = sb("Sb", [128, DEPTH, 4, 128], BF16)
        bSf = [[Buf(f"Sf{l}_{hp}") for hp in range(2)] for l in range(DEPTH)]
        bSb = [[Buf(f"Sb{l}_{hp}") for hp in range(2)] for l in range(DEPTH)]
        HL = sb("HL", [128, DEPTH, 2], F32)
        bHL = [[Buf(f"HL{l}_{c}") for c in range(2)] for l in range(DEPTH)]
        UH = sb("UH", [128, DEPTH, 2, 16], F32)
        bUH = [[Buf(f"UH{l}_{c}") for c in range(2)] for l in range(DEPTH)]
        XH = sb("XH", [128, DEPTH, 2, 4], F32)
        bXH = [[Buf(f"XH{l}_{c}") for c in range(2)] for l in range(DEPTH)]
        FH = sb("FH", [128, DEPTH, 48, 2], F32)
        bFH = [Buf(f"FH{l}") for l in range(DEPTH)]
        CORR = sb("CORR", [128, 48, 2], F32)
        bCORR = Buf("CORR")
        CTMP = sb("CTMP", [128, 48], F32)
        bCTMP = Buf("CTMP")
        NFM, NFF, NBM, NBF, NWM, NWF = 10, 5, 5, 4, 2, 3
        FRM = sb("FRM", [128, NFM, 528], F32)
        FRF = sb("FRF", [128, NFF, N], F32)
        BRM = sb("BRM", [128, NBM, N], BF16)
        BRF = sb("BRF", [128, NBF, N], BF16)
        WRM = sb("WRM", [128, NWM, 4096], BF16)
        WRF = sb("WRF", [128, NWF, 4096], BF16)
        PSB = [psum(f"PS{i}", [128, N], F32) for i in range(7)]
        PST = psum("PST", [128, 1024], BF16)
        bPST = Buf("PST")

        CM, CF = Cx(), Cx()
        CM.H, CM.bH = MIX, bMIX
        CM.fring = Ring("FM", NFM, lambda i: FRM[:, i, :])
        CM.bring = Ring("BM", NBM, lambda i: BRM[:, i, :])
        CM.pring = Ring("PM", 4, lambda i: PSB[i])
        CM.wring = Ring("WM", NWM, lambda i: i)
        CM.WR, CM.wname = WRM, "wm"
        CF.H, CF.bH = HF, bHF
        CF.fring = Ring("FF", NFF, lambda i: FRF[:, i, :])
        CF.bring = Ring("BF", NBF, lambda i: BRF[:, i, :])
        CF.pring = Ring("PF", 3, lambda i: PSB[4 + i])
        CF.wring = Ring("WF", NWF, lambda i: i)
        CF.WR, CF.wname = WRF, "wf"

        def ACT(out, in_, func, reads, writes, **kw):
            S.act(lambda e: e.activation(out=out, in_=in_, func=func, **kw), reads, writes)

        def MM(out, lhsT, rhs, start, stop, reads, writes):
            S.pe(lambda e: e.matmul(out, lhsT=lhsT, rhs=rhs, start=start, stop=stop), reads, writes)

        def STT(out, in0, scalar, in1, op0, op1, reads, writes):
            S.dve(lambda e: e.scalar_tensor_tensor(out=out, in0=in0, scalar=scalar, in1=in1, op0=op0, op1=op1),
                  reads, writes)

        def TT(eng, out, in0, in1, op, reads, writes):
            getattr(S, eng)(lambda e: e.tensor_tensor(out=out, in0=in0, in1=in1, op=op), reads, writes)

        def TS(eng, out, in0, s1, s2, op0, op1, reads, writes):
            if s2 is None:
                getattr(S, eng)(lambda e: e.tensor_scalar(out=out, in0=in0, scalar1=s1, scalar2=None, op0=op0),
                                reads, writes)
            else:
                getattr(S, eng)(lambda e: e.tensor_scalar(out=out, in0=in0, scalar1=s1, scalar2=s2, op0=op0, op1=op1),
                                reads, writes)

        def CP(eng, out, in_, reads, writes):
            if eng == "act":
                S.act(lambda e: e.activation(out=out, in_=in_, func=AF.Copy), reads, writes)
            else:
                getattr(S, eng)(lambda e: e.tensor_copy(out=out, in_=in_), reads, writes)

        def pcol(l, off, n=1):
            base = l * P_LAYER + off
            return PK[:, base:base + n]

        S.dma("sp", lambda e: e.dma_start(out=PK[:, :], in_=pk[:, :]), "pk", writes=[bPK])
        S.dma("sp", lambda e: e.dma_start(out=CN[:, :], in_=cn[:, :]), "cn", writes=[bCN])

        def load_x(t):
            ts_ = slice(t * N, (t + 1) * N)
            S.dma("sp", lambda e: e.dma_start(
                out=XTs[t % 2][:, :, :], in_=xT[:, ts_].rearrange("(k p) s -> p k s", p=128)),
                f"xl{t % 2}", writes=bXs[t % 2])

        load_x(0)
        load_x(1)
        for l in range(DEPTH):
            S.dma("pool", (lambda l: lambda e: e.dma_start(out=SM[:, l, :], in_=smat[l, :, :]))(l), "sm",
                  writes=[bSM], nodeps=True)
        bW16 = [[Buf(f"W16A_{l}", track=False), Buf(f"W16B_{l}", track=False)] for l in range(DEPTH)]
        for l in range(DEPTH):
            for s in range(NSLAB):
                o, wd = SLAB_OFF[s], SLAB_W[s]
                bb = 2048 if wd == 4096 else 1536
                grp = 0 if s < S_UP else 1
                S.dma("pool", (lambda l, o, wd, bb: lambda e: e.dma_start(
                    out=w16[l, :, o:o + wd].rearrange("p (a b) -> p a b", b=bb),
                    in_=w32[l, :, o:o + wd].rearrange("p (a b) -> p a b", b=bb)))(l, o, wd, bb),
                    f"cv{l}_{grp}", writes=[bW16[l][grp]], nodeps=True)
        S.dve(lambda e: e.memset(ONES[:, :], 1.0), writes=[bONES])
        S.dve(lambda e: e.tensor_copy(out=MASKB[:, :], in_=CN[:, C_MASK:C_MASK + 128]), reads=[bCN], writes=[bMASK])
        S.dve(lambda e: e.memset(IDF[:, :], 1.0), writes=[bIDF])
        S.pool(lambda e: e.affine_select(out=IDF[:, :], in_=IDF[:, :], pattern=[[1, 128]], base=0,
                                         channel_multiplier=-1, compare_op=ALU.is_equal, fill=0.0),
               reads=[bIDF], writes=[bIDF])
        S.pool(lambda e: e.tensor_copy(out=IDN[:, :], in_=IDF[:, :]), reads=[bIDF], writes=[bIDN])
        S.pool(lambda e: e.memset(KEZ[:, :, :], 0.0), writes=bKE)
        for (tl, bl) in ((Sf, bSf), (Sb, bSb)):
            S.pool((lambda tl: lambda e: e.memset(tl[:, :, :, :], 0.0))(tl), writes=[b for r in bl for b in r])
        S.pool(lambda e: e.memset(HL[:, :, :], 0.0), writes=[b for r in bHL for b in r])
        S.pool(lambda e: e.memset(UH[:, :, :, :], 0.0), writes=[b for r in bUH for b in r])
        S.pool(lambda e: e.memset(XH[:, :, :, :], 0.0), writes=[b for r in bXH for b in r])
        S.pool(lambda e: e.memset(FH[:, :, :, :], 0.0), writes=bFH)
        for l in range(DEPTH):
            TS("dve", DPK[:, l, 0:2], pcol(l, P_BG, 2), -1.0, None, ALU.mult, None, [bPK], [bDPK])
            fb, ft = CM.fring.get()
            ACT(ft[:, 0:2], pcol(l, P_LAM, 2), AF.Exp, [bPK], [fb], scale=-1.0)
            ACT(ft[:, 2:4], ft[:, 0:2], AF.Ln, [fb], [fb], bias=1.0)
            TS("dve", DPK[:, l, 2:4], ft[:, 2:4], -8.0, None, ALU.mult, None, [fb], [bDPK])

        def load_slab(cx, l, s):
            b, slot = cx.wring.get()
            o, wd = SLAB_OFF[s], SLAB_W[s]
            grp = 0 if s < S_UP else 1
            WRt = cx.WR
            S.dma("sp", lambda e: e.dma_start(out=WRt[:, slot, 0:wd], in_=w16[l, :, o:o + wd]),
                  f"{cx.wname}{slot}", reads=[bW16[l][grp]], writes=[b])
            return b, slot

        def rmsnorm(cx, X, bX):
            pb, pt = cx.pring.get()
            for k in range(8):
                qb, qt = cx.bring.get()
                ACT(qt, X[:, k, :], AF.Square, [bX[k]], [qb])
                MM(pt[:, :], ONES[:, :], qt, k == 0, k == 7, [bONES, qb], [pb])
            lb, lt = cx.fring.get()
            ACT(lt[:, 0:N], pt[:, :], AF.Ln, [pb], [lb], scale=1.0 / D, bias=EPS)
            rb, rt = cx.fring.get()
            ACT(rt[:, 0:N], lt[:, 0:N], AF.Exp, [lb], [rb], scale=-0.5)
            return rb, rt

        def norm_to_H(cx, X, bX, gcol_base):
            rb, rt = rmsnorm(cx, X, bX)
            for k in range(8):
                STT(cx.H[:, k, :], X[:, k, :], PK[:, gcol_base + k:gcol_base + k + 1], rt[:, 0:N],
                    ALU.mult, ALU.mult, [bX[k], bPK, rb], [cx.bH[k]])

        def norm_gen(cx, X, bX, gcol_base, ph):
            pb, pt = cx.pring.get()
            for k in range(8):
                qb, qt = cx.bring.get()
                ACT(qt, X[:, k, :], AF.Square, [bX[k]], [qb])
                MM(pt[:, :], ONES[:, :], qt, k == 0, k == 7, [bONES, qb], [pb])
                if k == 3:
                    yield ph
            yield ph
            lb, lt = cx.fring.get()
            ACT(lt[:, 0:N], pt[:, :], AF.Ln, [pb], [lb], scale=1.0 / D, bias=EPS)
            ACT(lt[:, 0:N], lt[:, 0:N], AF.Exp, [lb], [lb], scale=-0.5)
            yield ph
            for k in range(8):
                STT(cx.H[:, k, :], X[:, k, :], PK[:, gcol_base + k:gcol_base + k + 1], lt[:, 0:N],
                    ALU.mult, ALU.mult, [bX[k], bPK, lb], [cx.bH[k]])
                if k % 2 == 1:
                    yield ph

        def fm_group(cx, slot, wb, col0, M=128):
            pb, pt = cx.pring.get()
            for k in range(8):
                MM(pt[0:M, :], cx.WR[:, slot, k * 512 + col0:k * 512 + col0 + M], cx.H[:, k, :], k == 0, k == 7,
                   [wb, cx.bH[k]], [pb])
            return pb, pt

        pref = {}

        def mixer_gen(t, l, nxt=None):
            cx = CM
            X, bX = XTs[t % 2], bXs[t % 2]
            H, bH = cx.H, cx.bH
            fring, bring, pring = cx.fring, cx.bring, cx.pring
            first_tile = (t == 0)
            yield from norm_gen(cx, X, bX, l * P_LAYER + P_G1, 1)
            if ("M", t, l) in pref:
                wb, slot = pref.pop(("M", t, l))
            else:
                wb, slot = load_slab(cx, l, S_IN + 0)
            pb, pt = fm_group(cx, slot, wb, 256, M=16)
            gb_, GLOW = bring.get()
            ACT(GLOW[0:16, :], pt[0:16, :], AF.Copy, [pb], [gb_])
            yield 1
            cs = []
            for c in range(2):
                pb, pt = pring.get()
                MM(pt[:, :], SM[0:16, l, c * 128:(c + 1) * 128], GLOW[0:16, :], True, True, [bSM, gb_], [pb])
                eb_, et_ = fring.get()
                ACT(et_[:, 0:N], pt[:, :], AF.Exp, [pb, bDPK], [eb_], scale=-1.0, bias=DPK[:, l, c:c + 1])
                sb_, st_ = fring.get()
                ACT(st_[:, 0:N], et_[:, 0:N], AF.Ln, [eb_], [sb_], bias=1.0)
                cb_, ct_ = fring.get()
                S.dve((lambda ct_, st_: lambda e: e.tensor_tensor_scan(
                    out=ct_[:, 0:N], data0=CN[:, C_RESET:C_RESET + N], data1=st_[:, 0:N], initial=0.0,
                    op0=ALU.mult, op1=ALU.add))(ct_, st_), [sb_, bCN], [cb_])
                cs.append((cb_, ct_))
                yield 1
            ebs, enbs = [], []
            for c in range(2):
                cb_, ct_ = cs[c]
                b1, t1 = fring.get()
                ACT(t1[:, 0:N], ct_[:, 0:N], AF.Exp, [cb_], [b1], scale=-1.0 / 16)
                b2, t2 = fring.get()
                ACT(t2[:, 0:N], ct_[:, 0:N], AF.Exp, [cb_], [b2], scale=1.0 / 16)
                ACT(Dd[:, c, :], ct_[:, 127:N:128], AF.Exp, [cb_], [bDd[c]], scale=-1.0 / 16)
                ebs.append((b1, t1))
                enbs.append((b2, t2))
            for c in range(2):
                pb, pt = fm_group(cx, slot, wb, c * 128)
                ACT(GY[:, c, :], pt[:, :], AF.Gelu_apprx_tanh, [pb], [bGY[c]])
                yield 1
            wb, slot = load_slab(cx, l, S_IN + 1)
            for c in range(2):
                pb, pt = fm_group(cx, slot, wb, c * 128)
                STT(QE[:, c, :], pt[:, :], 0.125, ebs[c][1][:, 0:N], ALU.mult, ALU.mult, [pb, ebs[c][0]], [bQE[c]])
                yield 1
            for c in range(2):
                pb, pt = fm_group(cx, slot, wb, 256 + c * 128)
                ent = enbs[c][1]
                for a in range(2):
                    ps_ = slice(a * 64, (a + 1) * 64)
                    TT("dve", KEZ[ps_, 2 * c + a, :], pt[ps_, :], ent[ps_, 0:N], ALU.mult, [pb, enbs[c][0]], [bKE[c]])
                db_, dt_ = fring.get()
                for blk in range(4):
                    TS("dve", dt_[:, blk * 128:(blk + 1) * 128], ent[:, blk * 128:(blk + 1) * 128],
                       Dd[:, c, blk:blk + 1], None, ALU.mult, None, [enbs[c][0], bDd[c]], [db_])
                kb_, KDT = bring.get()
                TT("dve", KDT, pt[:, :], dt_[:, 0:N], ALU.mult, [pb, db_], [kb_])
                yield 1
                for blk in range(4):
                    S.pe((lambda KDT, blk: lambda e: e.transpose(
                        PST[:, blk * 128:(blk + 1) * 128], KDT[:, blk * 128:(blk + 1) * 128], IDN[:, :]))(KDT, blk),
                        [kb_, bIDN], [bPST])
                CP("act", KD_TM[:, c, :], PST[:, 0:N], [bPST], [bKD[c]])
                yield 1
            wb, slot = load_slab(cx, l, S_IN + 2)
            for blk in range(4):
                pb, pt = pring.get()
                for k in range(8):
                    MM(pt[:, :], H[:, k, blk * 128:(blk + 1) * 128], cx.WR[:, slot, k * 512:(k + 1) * 512],
                       k == 0, k == 7, [wb, bH[k]], [pb])
                CP("act", V_TM[:, blk, :], pt[:, :], [pb], [bV[blk]])
                yield 1
            wb, slot = load_slab(cx, l, S_IN + 3)
            for h in range(4):
                pb, pt = fm_group(cx, slot, wb, h * 128)
                fb, ft = fring.get()
                ACT(ft[:, 0:N], pt[:, :], AF.Silu, [pb], [fb])
                TS("dve", SILU[:, h, :], ft[:, 0:N], pcol(l, P_GN + h), None, ALU.mult, None, [fb, bPK], [bSILU[h]])
                yield 1
            wb, slot = load_slab(cx, l, S_IN + 4)
            for c in range(2):
                CP("pool", U[:, c, 0:16], UH[:, l, c, :], [bUH[l][c]], [bU[c]])
                pb, pt = fm_group(cx, slot, wb, c * 128)
                CP("act", U[:, c, 16:528], pt[:, :], [pb], [bU[c]])
                CP("pool", UH[:, l, c, :], U[:, c, 512:528], [bU[c]], [bUH[l][c]])
                yield 1
            for c in range(2):
                CP("pool", XR[:, c, 0:3], XH[:, l, c, 0:3], [bXH[l][c]], [bXR[c]])
                pb, pt = fm_group(cx, slot, wb, 256 + c * 128)
                CP("act", XR[:, c, 3:515], pt[:, :], [pb], [bXR[c]])
                CP("pool", XH[:, l, c, 0:3], XR[:, c, 512:515], [bXR[c]], [bXH[l][c]])
                yield 1

            psrc = []
            for c in range(2):
                u = U[:, c, :]
                b2, t2 = fring.get()
                TT("pool", t2[:, 1:528], u[:, 1:528], u[:, 0:527], ALU.add, [bU[c]], [b2])
                b4, t4 = fring.get()
                TT("pool", t4[:, 3:528], t2[:, 3:528], t2[:, 1:526], ALU.add, [b2], [b4])
                if c == 0:
                    psrc.append(((b2, t2, 2, 0), (b4, t4, 4, 1)))
                else:
                    b8, t8 = fring.get()
                    TT("pool", t8[:, 7:528], t4[:, 7:528], t4[:, 3:524], ALU.add, [b4], [b8])
                    b16, t16 = fring.get()
                    TT("pool", t16[:, 15:528], t8[:, 15:528], t8[:, 7:520], ALU.add, [b8], [b16])
                    psrc.append(((b8, t8, 8, 2), (b16, t16, 16, 3)))
            xcs = []
            for c in range(2):
                xb_, xc = fring.get()
                TS("pool", xc[:, 0:N], XR[:, c, 3:515], pcol(l, P_LCW + c * 4 + 3), pcol(l, P_LCB + c),
                   ALU.mult, ALU.add, [bXR[c], bPK], [xb_])
                xcs.append((xb_, xc))
            yield 2
            yield 2
            yield 2
            dpls = []
            if first_tile:
                fb, ft = fring.get()
            for c in range(2):
                u = U[:, c, :]
                dpb, DPLc = bring.get()
                for a, (sbuf_, stile, win, wi) in enumerate(psrc[c]):
                    ps_ = slice(a * 64, (a + 1) * 64)
                    STT(DPLc[ps_, :], stile[ps_, 16:528], 1.0 / win, u[ps_, 16:528], ALU.mult, ALU.subtract,
                        [sbuf_, bU[c]], [dpb])
                    if first_tile:
                        fcol = slice(c * 16, (c + 1) * 16)
                        TT("dve", ft[ps_, fcol], stile[ps_, 16:32], CN[ps_, C_INV + wi * 16:C_INV + (wi + 1) * 16],
                           ALU.mult, [sbuf_, bCN], [fb])
                        TT("dve", DPLc[ps_, 0:16], ft[ps_, fcol], u[ps_, 16:32], ALU.subtract,
                           [fb, bU[c], dpb], [dpb])
                dpls.append((dpb, DPLc))
            for c in range(2):
                xb_, xc = xcs[c]
                for k in range(3):
                    STT(xc[:, 0:N], XR[:, c, k:k + N], pcol(l, P_LCW + c * 4 + k), xc[:, 0:N], ALU.mult, ALU.add,
                        [bXR[c], bPK, xb_], [xb_])
            yield 2
            yield 2
            cbs = []
            for c in range(2):
                cbb, cbt = bring.get()
                CP("pool", cbt, xcs[c][1][:, 0:N], [xcs[c][0]], [cbb])
                cbs.append((cbb, cbt))
            pps = []
            for c in range(2):
                pb, pt = pring.get()
                MM(pt[:, :], SM[:, l, 256 + c * 128:256 + (c + 1) * 128], dpls[c][1], True, True, [bSM, dpls[c][0]], [pb])
                pps.append((pb, pt))
            yield 2
            yield 2
            for c in range(2):
                pb, pt = pps[c]
                ACT(MIX[:, 4 + c, :], pt[:, :], AF.Identity, [pb, bPK], [bMIX[4 + c]], scale=pcol(l, P_PSC + c))
            lr = []
            for c in range(2):
                cbb, cbt = cbs[c]
                pa, pat = pring.get()
                MM(pat[:, :], SM[:, l, 512 + c * 128:512 + (c + 1) * 128], cbt, True, True, [bSM, cbb], [pa])
                pi, pit = pring.get()
                MM(pit[:, :], SM[:, l, 768 + c * 128:768 + (c + 1) * 128], cbt, True, True, [bSM, cbb], [pi])
                yield 2
                rb_, rt_ = fring.get()
                ACT(rt_[:, 0:N], pat[:, :], AF.Sigmoid, [pa, bPK], [rb_], bias=pcol(l, P_LBA + c))
                ib_, it_ = fring.get()
                ACT(it_[:, 0:N], pit[:, :], AF.Sigmoid, [pi, bPK], [ib_], bias=pcol(l, P_LBX + c))
                lr.append((rb_, rt_, ib_, it_))
            yield 2
            for c in range(2):
                rb_, rt_, ib_, it_ = lr[c]
                ACT(rt_[:, 0:N], rt_[:, 0:N], AF.Exp, [rb_, bDPK], [rb_], scale=DPK[:, l, 2 + c:3 + c])
                TT("pool", it_[:, 0:N], it_[:, 0:N], xcs[c][1][:, 0:N], ALU.mult, [ib_, xcs[c][0]], [ib_])
            yield 2
            qs = []
            for c in range(2):
                rb_, rt_, ib_, it_ = lr[c]
                a2b, a2t = fring.get()
                TT("dve", a2t[:, 0:N], rt_[:, 0:N], rt_[:, 0:N], ALU.mult, [rb_], [a2b])
                qs.append((a2b, a2t))
            yield 2
            for c in range(2):
                a2b, a2t = qs[c]
                ACT(a2t[:, 0:N], a2t[:, 0:N], AF.Sqrt, [a2b], [a2b], scale=-1.0, bias=1.0)
            yield 2
            yield 2
            for c in range(2):
                rb_, rt_, ib_, it_ = lr[c]
                a2b, a2t = qs[c]
                xb_, xc = xcs[c]
                TT("dve", a2t[:, 0:N], a2t[:, 0:N], it_[:, 0:N], ALU.mult, [a2b, ib_], [a2b])
                S.dve((lambda xc, rt_, a2t, c: lambda e: e.tensor_tensor_scan(
                    out=xc[:, 0:N], data0=rt_[:, 0:N], data1=a2t[:, 0:N], initial=HL[:, l, c:c + 1],
                    op0=ALU.mult, op1=ALU.add))(xc, rt_, a2t, c), [rb_, a2b, bHL[l][c]], [xb_])
                CP("dve", HL[:, l, c:c + 1], xc[:, N - 1:N], [xb_], [bHL[l][c]])
                TT("dve", MIX[:, 6 + c, :], xc[:, 0:N], GY[:, c, :], ALU.mult, [xb_, bGY[c]], [bMIX[6 + c]])
            yield 2

            g_sc, g_kv, g_st, g_ot, g_q, g_pre, g_ss, g_rt = {}, {}, {}, {}, {}, {}, {}, {}

            def gla_stage(s, blk):
                bs = slice(blk * 128, (blk + 1) * 128)
                if s == 0:
                    pb, pt = pring.get()
                    for h in range(4):
                        hp, a = divmod(h, 2)
                        MM(pt[:, h * 128:(h + 1) * 128], KEZ[:, h, bs], QE[:, hp, bs], True, True,
                           [bKE[hp], bQE[hp]], [pb])
                    g_sc[blk] = (pb, pt)
                    kb, kt = pring.get()
                    for hp in range(2):
                        MM(kt[:, hp * 256:(hp + 1) * 256], KD_TM[:, hp, bs], V_TM[:, blk, hp * 256:(hp + 1) * 256],
                           True, True, [bKD[hp], bV[blk]], [kb])
                    g_kv[blk] = (kb, kt)
                elif s == 1:
                    pb, pt = g_sc[blk]
                    sb_, st_ = scring.get()
                    TT("dve", st_.rearrange("p (h n) -> p h n", h=4), pt[:, :].rearrange("p (h n) -> p h n", h=4),
                       MASKB[:, :].unsqueeze(1).to_broadcast([128, 4, 128]), ALU.mult, [pb, bMASK], [sb_])
                    g_st[blk] = (sb_, st_)
                    kb, kt = g_kv[blk]
                    for hp in range(2):
                        for a in range(2):
                            ps_ = slice(a * 64, (a + 1) * 64)
                            STT(Sf[ps_, l, hp, :], Sf[ps_, l, hp, :], Dd[ps_, hp, blk:blk + 1],
                                kt[ps_, hp * 256 + a * 128:hp * 256 + (a + 1) * 128], ALU.mult, ALU.add,
                                [bSf[l][hp], bDd[hp], kb], [bSf[l][hp]])
                elif s == 2:
                    sb_, st_ = g_st[blk]
                    ob, ot = pring.get()
                    for h in range(4):
                        hp, a = divmod(h, 2)
                        MM(ot[:, h * 128:(h + 1) * 128], V_TM[:, blk, h * 128:(h + 1) * 128],
                           st_[:, h * 128:(h + 1) * 128], True, False, [bV[blk], sb_], [ob])
                        MM(ot[:, h * 128:(h + 1) * 128], Sb[:, l, h, :], QE[:, hp, bs], False, True,
                           [bSb[l][hp], bQE[hp]], [ob])
                    g_ot[blk] = (ob, ot)
                    for hp in range(2):
                        for a in range(2):
                            ps_ = slice(a * 64, (a + 1) * 64)
                            CP("act", Sb[ps_, l, 2 * hp + a, :], Sf[ps_, l, hp, :], [bSf[l][hp]], [bSb[l][hp]])
                elif s == 3:
                    ob, ot = g_ot[blk]
                    qb, qt = bring.get()
                    ACT(qt, ot[:, :], AF.Square, [ob], [qb])
                    g_q[blk] = (qb, qt)
                    tb, tt = fring.get()
                    TT("dve", tt[:, 0:N].rearrange("p (h n) -> p h n", h=4),
                       ot[:, :].rearrange("p (h n) -> p h n", h=4), SILU[:, :, bs], ALU.mult, [ob] + bSILU, [tb])
                    g_pre[blk] = (tb, tt)
                elif s == 4:
                    qb, qt = g_q[blk]
                    nb_, nt_ = pring.get()
                    MM(nt_[:, :], ONES[:, :], qt, True, True, [bONES, qb], [nb_])
                    g_ss[blk] = (nb_, nt_)
                elif s == 5:
                    nb_, nt_ = g_ss[blk]
                    lb, lt = fring.get()
                    ACT(lt[:, 0:N], nt_[:, :], AF.Ln, [nb_], [lb], scale=1.0 / 128, bias=EPS)
                    ACT(lt[:, 0:N], lt[:, 0:N], AF.Exp, [lb], [lb], scale=-0.5)
                    g_rt[blk] = (lb, lt)
                elif s == 6:
                    tb, tt = g_pre[blk]
                    lb, lt = g_rt[blk]
                    TT("dve", MIX[:, 0:4, bs], tt[:, 0:N].rearrange("p (h n) -> p h n", h=4),
                       lt[:, 0:N].rearrange("p (h n) -> p h n", h=4), ALU.mult, [tb, lb], bMIX[0:4])

            for slot in range(2 * 3 + 7):
                for s in range(6, -1, -1):
                    if (slot - s) % 2 == 0 and 0 <= (slot - s) // 2 < 4:
                        gla_stage(s, (slot - s) // 2)
                yield 2

            slabs = [load_slab(cx, l, S_OUT + i) for i in range(2)]
            for n in range(8):
                if n == 4 and nxt is not None:
                    pref[("M",) + nxt] = load_slab(cx, nxt[1], S_IN + 0)
                wb, slot = slabs[n // 4]
                col0 = (n % 4) * 128
                pb, pt = pring.get()
                for k in range(8):
                    MM(pt[:, :], cx.WR[:, slot, k * 512 + col0:k * 512 + col0 + 128], MIX[:, k, :], k == 0, k == 7,
                       [wb, bMIX[k]], [pb])
                TT("dve", X[:, n, :], X[:, n, :], pt[:, :], ALU.add, [bX[n], pb], [bX[n]])
                yield 3

        def ffn_pre(t, l):
            cx = CF
            X, bX = XTs[t % 2], bXs[t % 2]
            yield from norm_gen(cx, X, bX, l * P_LAYER + P_G2, 4)
            fcw = PK[:, l * P_LAYER + P_FCW:l * P_LAYER + P_FCW + 144].rearrange("p (g k) -> p g k", k=3)
            TT("dve", CORR[:, :, 1], FH[:, l, :, 1], fcw[:, :, 0], ALU.mult, [bFH[l], bPK], [bCORR])
            TT("dve", CORR[:, :, 0], FH[:, l, :, 1], fcw[:, :, 1], ALU.mult, [bFH[l], bPK], [bCORR])
            TT("dve", CTMP[:, :], FH[:, l, :, 0], fcw[:, :, 0], ALU.mult, [bFH[l], bPK], [bCTMP])
            TT("dve", CORR[:, :, 0], CORR[:, :, 0], CTMP[:, :], ALU.add, [bCORR, bCTMP], [bCORR])
            yield 4

        def ffn_gen(t, l, nxt=None):
            cx = CF
            X, bX = XTs[t % 2], bXs[t % 2]
            fring, bring, pring = cx.fring, cx.bring, cx.pring
            if not EARLY_PRE:
                yield from ffn_pre(t, l)

            def conv_group(slot, wb, col0, g):
                pb, pt = fm_group(cx, slot, wb, col0)
                ab_, at_ = fring.get()
                base = l * P_LAYER + P_FCW + g * 3
                ACT(at_[:, 0:N], pt[:, :], AF.Identity, [pb, bPK], [ab_], scale=PK[:, base + 2:base + 3],
                    bias=pcol(l, P_FCB + g))
                STT(at_[:, 1:N], pt[:, 0:N - 1], PK[:, base + 1:base + 2], at_[:, 1:N], ALU.mult, ALU.add,
                    [pb, bPK, ab_], [ab_])
                STT(at_[:, 2:N], pt[:, 0:N - 2], PK[:, base:base + 1], at_[:, 2:N], ALU.mult, ALU.add,
                    [pb, bPK, ab_], [ab_])
                TT("dve", at_[:, 0:2], at_[:, 0:2], CORR[:, g, :], ALU.add, [ab_, bCORR], [ab_])
                CP("act", FH[:, l, g, :], pt[:, N - 2:N], [pb], [bFH[l]])
                return ab_, at_

            def gating(fch, gb, gt, vb, vt):
                ggb, ggt = bring.get()
                ACT(ggt, gt[:, 0:N], AF.Gelu_apprx_tanh, [gb], [ggb])
                TT("pool", A24[:, fch, :], ggt, vt[:, 0:N], ALU.mult, [ggb, vb], [bA24[fch]])

            pend = None
            for i in range(6):
                if i == 0 and ("F", t, l) in pref:
                    (wg, sg), (wv, sv) = pref.pop(("F", t, l))
                else:
                    wg, sg = load_slab(cx, l, S_UP + 2 * i)
                    wv, sv = load_slab(cx, l, S_UP + 2 * i + 1)
                for j in range(4):
                    fch = 4 * i + j
                    gb, gt = conv_group(sg, wg, j * 128, fch)
                    yield 8
                    vb, vt = conv_group(sv, wv, j * 128, 24 + fch)
                    if pend is not None:
                        gating(*pend)
                    pend = (fch, gb, gt, vb, vt)
                    yield 8
            gating(*pend)
            for n in range(8):
                wb, slot = load_slab(cx, l, S_DN + n)
                if n == 7 and nxt is not None:
                    pref[("F",) + nxt] = [load_slab(cx, nxt[1], S_UP + 0), load_slab(cx, nxt[1], S_UP + 1)]
                pb, pt = pring.get()
                for j in range(24):
                    MM(pt[:, :], cx.WR[:, slot, j * 128:(j + 1) * 128], A24[:, j, :], j == 0, j == 23,
                       [wb, bA24[j]], [pb])
                    if j % 8 == 7:
                        yield 8
                TT("dve", X[:, n, :], X[:, n, :], pt[:, :], ALU.add, [bX[n], pb], [bX[n]])
            if l == DEPTH - 1:
                rb, rt = rmsnorm(cx, X, bX)
                gb = DEPTH * P_LAYER
                for k in range(8):
                    STT(X[:, k, :], X[:, k, :], PK[:, gb + k:gb + k + 1], rt[:, 0:N], ALU.mult, ALU.mult,
                        [bX[k], bPK, rb], [bX[k]])
                ts_ = slice(t * N, (t + 1) * N)
                S.dma("act", lambda e: e.dma_start(
                    out=outT[:, ts_].rearrange("(k p) s -> p k s", p=128), in_=X[:, :, :]), f"st{t % 2}", reads=bX)
                if t + 2 < NT:
                    load_x(t + 2)
                yield 8

        TL = [(t, l) for pair in range(NT // 2) for l in range(DEPTH) for t in (2 * pair, 2 * pair + 1)]
        def mside(t, l, nxt=None):
            yield from mixer_gen(t, l, nxt)
            if EARLY_PRE:
                yield from ffn_pre(t, l)

        S.dry = True
        cnt = {1: 0, 2: 0, 3: 0, 4: 0}
        for ph in mside(2, 0):
            cnt[ph] += 1
        S.dry = False
        xs = [0]
        for ph in ((1, 2, 3, 4) if EARLY_PRE else (1, 2, 3)):
            xs.append(xs[-1] + cnt[ph])
        ys = M_SCHED_Y if EARLY_PRE else M_SCHED_Y3

        def m_target(i):
            for j in range(1, len(xs)):
                if i <= xs[j]:
                    return ys[j - 1] + (ys[j] - ys[j - 1]) * (i - xs[j - 1]) / float(xs[j] - xs[j - 1])
            return ys[-1]

        for step in range(len(TL) + 1):
            nxt_m = TL[step + 1] if step + 1 < len(TL) else None
            nxt_f = TL[step] if step < len(TL) else None
            gm = mside(*TL[step], nxt=nxt_m) if step < len(TL) else None
            gf = ffn_gen(*TL[step - 1], nxt=nxt_f) if step >= 1 else None
            pm = 0
            pf = 0.0
            while gm is not None or gf is not None:
                run_m = gm is not None and (gf is None or m_target(pm) <= pf / FFN_W)
                if run_m:
                    try:
                        next(gm)
                        pm += 1
                    except StopIteration:
                        gm = None
                else:
                    try:
                        pf += next(gf)
                    except StopIteration:
                        gf = None
        global _SBUF_LEFT
        _SBUF_LEFT = nc.sbuf_bytes_remaining
        S.emit(final_waits=["st0", "st1"])
    return nc


MIXER_W = 54.0
EARLY_PRE = False
M_SCHED_Y = (0.02, 0.36, 0.80, 0.86, 0.95)
M_SCHED_Y3 = (0.0, 0.42, 0.93, 1.0)
FFN_W = 8.0 * (48 + 24)


def _slab_k(Wc):
    return np.ascontiguousarray(Wc.reshape(8, 128, 512).transpose(1, 0, 2)).reshape(128, 4096)


def _fm(v, n):
    return np.ascontiguousarray(v.reshape(n, 128).T)


def prep_host(inp, DEPTH):
    w32 = np.zeros((DEPTH, 128, WCOLS), np.float32)
    smat = np.zeros((DEPTH, 128, 1024), np.float32)
    pk = np.zeros((128, DEPTH * P_LAYER + 8), np.float32)
    for l in range(DEPTH):
        wi = inp["w_in"][l]
        q, k, v, g = wi[:, 0:256], wi[:, 256:512], wi[:, 512:1024], wi[:, 1024:1536]
        glow, pu, lx, ly = wi[:, 1536:1552], wi[:, 1552:1808], wi[:, 1808:2064], wi[:, 2064:2320]
        z = np.zeros((1024, 240), np.float32)
        slabs = [np.concatenate([ly, glow, z], 1), np.concatenate([q, k], 1), v, g, np.concatenate([pu, lx], 1)]
        wo = inp["w_out"][l]
        slabs += [wo[:, 0:512], wo[:, 512:1024]]
        wu = inp["ffn_w_up"][l]
        for i in range(6):
            slabs += [wu[:, 512 * i:512 * (i + 1)], wu[:, 3072 + 512 * i:3072 + 512 * (i + 1)]]
        for s, sl in enumerate(slabs):
            w32[l, :, SLAB_OFF[s]:SLAB_OFF[s + 1]] = _slab_k(sl)
        wd = inp["ffn_w_down"][l]
        for n in range(8):
            blk = wd[:, n * 128:(n + 1) * 128].reshape(24, 128, 128).transpose(1, 0, 2).reshape(128, 3072)
            s = S_DN + n
            w32[l, :, SLAB_OFF[s]:SLAB_OFF[s + 1]] = blk
        smat[l, 0:16, 0:256] = inp["gla_wg2"][l]
        for c in range(2):
            for a in range(2):
                r = slice(a * 64, (a + 1) * 64)
                smat[l, r, 256 + c * 128 + a * 64:256 + c * 128 + (a + 1) * 64] = inp["pool_w"][l, 2 * c + a]
                smat[l, r, 512 + c * 128 + a * 64:512 + c * 128 + (a + 1) * 64] = inp["lru_wa"][l, 2 * c + a]
                smat[l, r, 768 + c * 128 + a * 64:768 + c * 128 + (a + 1) * 64] = inp["lru_wx"][l, 2 * c + a]
        b = l * P_LAYER
        pk[:, b + P_G1:b + P_G1 + 8] = _fm(inp["norm1_g"][l], 8)
        pk[:, b + P_G2:b + P_G2 + 8] = _fm(inp["norm2_g"][l], 8)
        pk[:, b + P_BG:b + P_BG + 2] = _fm(inp["gla_bg"][l], 2)
        pk[:, b + P_GN:b + P_GN + 4] = inp["gla_norm_g"][l].T
        pk[:, b + P_PSC:b + P_PSC + 2] = _fm(inp["pool_scale"][l], 2)
        for c in range(2):
            pk[:, b + P_LCW + c * 4:b + P_LCW + c * 4 + 4] = inp["lru_conv_w"][l][:, c * 128:(c + 1) * 128].T
        pk[:, b + P_LCB:b + P_LCB + 2] = _fm(inp["lru_conv_b"][l], 2)
        pk[:, b + P_LBA:b + P_LBA + 2] = _fm(inp["lru_ba"][l], 2)
        pk[:, b + P_LBX:b + P_LBX + 2] = _fm(inp["lru_bx"][l], 2)
        pk[:, b + P_LAM:b + P_LAM + 2] = _fm(inp["lru_lambda"][l], 2)
        fw = inp["ffn_conv_w"][l]
        pk[:, b + P_FCW:b + P_FCW + 144] = fw.reshape(3, 48, 128).transpose(2, 1, 0).reshape(128, 144)
        pk[:, b + P_FCB:b + P_FCB + 48] = _fm(inp["ffn_conv_b"][l], 48)
    pk[:, DEPTH * P_LAYER:DEPTH * P_LAYER + 8] = _fm(inp["final_g"], 8)
    cn = np.zeros((128, NCN), np.float32)
    j = np.arange(128)[:, None]
    i = np.arange(128)[None, :]
    cn[:, C_MASK:C_MASK + 128] = (j <= i)
    rs = np.ones(512, np.float32)
    rs[::128] = 0.0
    cn[:, C_RESET:C_RESET + 512] = rs[None, :]
    for wi_, win in enumerate((2, 4, 8, 16)):
        cn[:, C_INV + wi_ * 16:C_INV + (wi_ + 1) * 16] = (1.0 / np.minimum(np.arange(1, 17), win))[None, :]
    return w32, smat, pk, cn


_NC_CACHE = {}


def run(inp, S_len, DEPTH, n_cores=8):
    key = (S_len, DEPTH)
    if key not in _NC_CACHE:
        _NC_CACHE[key] = build_nc(S_len, DEPTH)
    nc = _NC_CACHE[key]
    w32, smat, pk, cn = prep_host(inp, DEPTH)
    x = inp["x"]
    in_maps = []
    for b in range(n_cores):
        in_maps.append({"xT": np.ascontiguousarray(x[b].T), "w32": w32, "smat": smat, "pk": pk, "cn": cn})
    res = run_bass_kernel_spmd(nc, in_maps, core_ids=list(range(n_cores)))
    out = np.stack([np.ascontiguousarray(res.results[b]["outT"].T) for b in range(n_cores)], 0)
    return out.astype(np.float32)


def kernel(**inputs):
    inp = {k: np.asarray(v) for k, v in inputs.items()}
    return run(inp, 4096, 4, 8)
```

```python
import numpy as np
from contextlib import ExitStack
import concourse.bass as bass
import concourse.mybir as mybir
from concourse.bass_utils import run_bass_kernel_spmd

F32 = mybir.dt.float32
BF16 = mybir.dt.bfloat16
AF = mybir.ActivationFunctionType
ALU = mybir.AluOpType

D = 1024
NT_TOK = 512
EPS = 1e-6
N_IN_SLABS, N_OUT_SLABS, N_UP_SLABS, N_DN_SLABS = 5, 2, 12, 8
SLAB_W = [4096] * (N_IN_SLABS + N_OUT_SLABS + N_UP_SLABS) + [3072] * N_DN_SLABS
SLAB_OFF = [0]
for _w in SLAB_W:
    SLAB_OFF.append(SLAB_OFF[-1] + _w)
WCOLS = SLAB_OFF[-1]
NSLAB = len(SLAB_W)
S_IN, S_OUT, S_UP, S_DN = 0, N_IN_SLABS, N_IN_SLABS + N_OUT_SLABS, N_IN_SLABS + N_OUT_SLABS + N_UP_SLABS

P_G1, P_G2, P_BG, P_GN, P_PSC, P_LCW, P_LCB, P_LBA, P_LBX, P_LAM, P_FCW, P_FCB = (
    0, 8, 16, 18, 22, 24, 32, 34, 36, 38, 40, 184)
P_LAYER = 232
C_MASK, C_RESET, C_INV = 0, 128, 640
NCN = 704

ENGS = ("pe", "act", "dve", "pool", "sp")


class Buf:
    __slots__ = ("name", "w", "r", "track")

    def __init__(self, name, track=True):
        self.name = name
        self.w = None
        self.r = {}
        self.track = track


class Sched:
    def __init__(self, nc):
        self.nc = nc
        self.ops = {e: [] for e in ENGS}
        self.dma_sems = {}
        self.dry = False

    def _add(self, eng, fn, reads, writes, dma_sem=None, nodeps=False):
        if self.dry:
            return None
        idx = len(self.ops[eng])
        keep = []
        if not nodeps:
            deps = []
            for b in reads:
                if b.w is not None:
                    deps.append((b.w, "raw"))
            for b in writes:
                if b.w is not None:
                    deps.append((b.w, "waw"))
                for tk in b.r.values():
                    deps.append((tk, "war"))
            for tk, kind in deps:
                if tk[0] == "E" and tk[1] == eng and dma_sem is None and eng == "pe":
                    continue
                keep.append(tk)
        if dma_sem is None:
            tok = ("E", eng, idx)
        else:
            ent = self.dma_sems.setdefault(dma_sem, [None, 0])
            ent[1] += 16
            tok = ("D", dma_sem, ent[1])
        for b in reads:
            if b.track:
                old = b.r.get(tok[1])
                if old is None or old[2] < tok[2]:
                    b.r[tok[1]] = tok
        for b in writes:
            b.w = tok
            b.r = {}
        self.ops[eng].append({"fn": fn, "deps": keep, "dma": dma_sem})
        return tok

    def pe(self, fn, reads=(), writes=()):
        return self._add("pe", fn, reads, writes)

    def act(self, fn, reads=(), writes=()):
        return self._add("act", fn, reads, writes)

    def dve(self, fn, reads=(), writes=()):
        return self._add("dve", fn, reads, writes)

    def pool(self, fn, reads=(), writes=()):
        return self._add("pool", fn, reads, writes)

    def dma(self, queue, fn, sem, reads=(), writes=(), nodeps=False):
        return self._add(queue, fn, reads, writes, dma_sem=sem, nodeps=nodeps)

    def emit(self, final_waits=()):
        nc = self.nc
        signaled = {e: set() for e in ENGS}
        for e in ENGS:
            for op in self.ops[e]:
                for tk in op["deps"]:
                    if tk[0] == "E":
                        signaled[tk[1]].add(tk[2])
        cum = {}
        for e in ENGS:
            c = 0
            m = {}
            for i in range(len(self.ops[e])):
                if i in signaled[e]:
                    c += 1
                    m[i] = c
            cum[e] = m
        with ExitStack() as es:
            esem = {e: es.enter_context(nc.semaphore("s_" + e)) for e in ENGS}
            for name, ent in self.dma_sems.items():
                ent[0] = es.enter_context(nc.semaphore("d_" + name))
            block = es.enter_context(nc.Block())

            def run(ename, eng):
                waited = {}
                for i, op in enumerate(self.ops[ename]):
                    need = {}
                    for tk in op["deps"]:
                        if tk[0] == "E":
                            key = ("E", tk[1])
                            val = cum[tk[1]][tk[2]]
                        else:
                            key = ("D", tk[1])
                            val = tk[2]
                        if need.get(key, 0) < val:
                            need[key] = val
                    for key, val in need.items():
                        if waited.get(key, 0) >= val:
                            continue
                        waited[key] = val
                        sem = esem[key[1]] if key[0] == "E" else self.dma_sems[key[1]][0]
                        eng.wait_ge(sem, val)
                    ins = op["fn"](eng)
                    if op["dma"] is not None:
                        ins.then_inc(self.dma_sems[op["dma"]][0], 16)
                    elif i in signaled[ename]:
                        ins.then_inc(esem[ename], 1)
                if ename == "act":
                    for name in final_waits:
                        ent = self.dma_sems[name]
                        eng.wait_ge(ent[0], ent[1])

            @block.sync
            def _(eng):
                run("sp", eng)

            @block.tensor
            def _(eng):
                run("pe", eng)

            @block.scalar
            def _(eng):
                run("act", eng)

            @block.vector
            def _(eng):
                run("dve", eng)

            @block.gpsimd
            def _(eng):
                run("pool", eng)


class Ring:
    def __init__(self, name, n, apf):
        self.bufs = [Buf(f"{name}{i}") for i in range(n)]
        self.apf = apf
        self.n = n
        self.i = 0

    def get(self):
        i = self.i
        self.i = (i + 1) % self.n
        return self.bufs[i], self.apf(i)


class Cx:
    pass


def build_nc(S_len, DEPTH):
    NT = S_len // NT_TOK
    assert NT >= 2 and NT % 2 == 0
    N = NT_TOK
    NPK = DEPTH * P_LAYER + 8
    nc = bass.Bass("TRN2", target_bir_lowering=False)
    xT = nc.dram_tensor("xT", [D, S_len], F32, kind="ExternalInput").ap()
    w32 = nc.dram_tensor("w32", [DEPTH, 128, WCOLS], F32, kind="ExternalInput").ap()
    smat = nc.dram_tensor("smat", [DEPTH, 128, 1024], F32, kind="ExternalInput").ap()
    pk = nc.dram_tensor("pk", [128, NPK], F32, kind="ExternalInput").ap()
    cn = nc.dram_tensor("cn", [128, NCN], F32, kind="ExternalInput").ap()
    outT = nc.dram_tensor("outT", [D, S_len], F32, kind="ExternalOutput").ap()
    w16 = nc.dram_tensor("w16", [DEPTH, 128, WCOLS], BF16, kind="Internal").ap()
    S = Sched(nc)

    with ExitStack() as es:
        def sb(name, shape, dt):
            return es.enter_context(nc.sbuf_tensor(name, shape, dt))

        def psum(name, shape, dt):
            return es.enter_context(nc.psum_tensor(name, shape, dt))

        XTs = [sb(f"XT{i}", [128, 8, N], F32) for i in range(2)]
        bXs = [[Buf(f"X{i}_{k}") for k in range(8)] for i in range(2)]
        MIX = sb("MIX", [128, 8, N], BF16)
        bMIX = [Buf(f"MIX{k}") for k in range(8)]
        HF = sb("HF", [128, 8, N], BF16)
        bHF = [Buf(f"HF{k}") for k in range(8)]
        A24 = sb("A24", [128, 24, N], BF16)
        bA24 = [Buf(f"A24_{j}") for j in range(24)]
        PK = sb("PK", [128, NPK], F32)
        bPK = Buf("PK", track=False)
        DPK = sb("DPK", [128, DEPTH, 4], F32)
        bDPK = Buf("DPK", track=False)
        SM = sb("SM", [128, DEPTH, 1024], BF16)
        bSM = Buf("SM", track=False)
        CN = sb("CN", [128, NCN], F32)
        bCN = Buf("CN", track=False)
        MASKB = sb("MASKB", [128, 128], BF16)
        ONES = sb("ONES", [128, 128], BF16)
        IDN = sb("IDN", [128, 128], BF16)
        IDF = sb("IDF", [128, 128], F32)
        bONES = Buf("ONES", track=False)
        bMASK = Buf("MASK", track=False)
        bIDN = Buf("IDN", track=False)
        bIDF = Buf("IDF")
        V_TM = sb("V_TM", [128, 4, N], BF16)
        bV = [Buf(f"V{b}") for b in range(4)]
        KD_TM = sb("KD_TM", [128, 2, N], BF16)
        bKD = [Buf(f"KD{c}") for c in range(2)]
        QE = sb("QE", [128, 2, N], BF16)
        bQE = [Buf(f"QE{c}") for c in range(2)]
        KEZ = sb("KEZ", [128, 4, N], BF16)
        bKE = [Buf(f"KE{c}") for c in range(2)]
        SILU = sb("SILU", [128, 4, N], BF16)
        bSILU = [Buf(f"SILU{h}") for h in range(4)]
        GY = sb("GY", [128, 2, N], BF16)
        bGY = [Buf(f"GY{c}") for c in range(2)]
        U = sb("U", [128, 2, 528], F32)
        bU = [Buf(f"U{c}") for c in range(2)]
        XR = sb("XR", [128, 2, 516], F32)
        bXR = [Buf(f"XR{c}") for c in range(2)]
        SCM = sb("SCM", [128, 2, N], BF16)
        scring = Ring("SCM", 2, lambda i: SCM[:, i, :])
        Dd = sb("Dd", [128, 2, 4], F32)
        bDd = [Buf(f"Dd{c}") for c in range(2)]
        Sf = sb("Sf", [128, DEPTH, 2, 128], F32)
        Sb = sb("Sb", [128, DEPTH, 4, 128], BF16)
        bSf = [[Buf(f"Sf{l}_{hp}") for hp in range(2)] for l in range(DEPTH)]
        bSb = [[Buf(f"Sb{l}_{hp}") for hp in range(2)] for l in range(DEPTH)]
        HL = sb("HL", [128, DEPTH, 2], F32)
        bHL = [[Buf(f"HL{l}_{c}") for c in range(2)] for l in range(DEPTH)]
        UH = sb("UH", [128, DEPTH, 2, 16], F32)
        bUH = [[Buf(f"UH{l}_{c}") for c in range(2)] for l in range(DEPTH)]
        XH = sb("XH", [128, DEPTH, 2, 4], F32)
        bXH = [[Buf(f"XH{l}_{c}") for c in range(2)] for l in range(DEPTH)]
        FH = sb("FH", [128, DEPTH, 48, 2], F32)
        bFH = [Buf(f"FH{l}") for l in range(DEPTH)]
        CORR = sb("CORR", [128, 48, 2], F32)
        bCORR = Buf("CORR")
        CTMP = sb("CTMP", [128, 48], F32)
        bCTMP = Buf("CTMP")
        NFM, NFF, NBM, NBF, NWM, NWF = 10, 5, 5, 4, 2, 3
        FRM = sb("FRM", [128, NFM, 528], F32)
        FRF = sb("FRF", [128, NFF, N], F32)
        BRM = sb("BRM", [128, NBM, N], BF16)
        BRF = sb("BRF", [128, NBF, N], BF16)
        WRM = sb("WRM", [128, NWM, 4096], BF16)
        WRF = sb("WRF", [128, NWF, 4096], BF16)
        PSB = [psum(f"PS{i}", [128, N], F32) for i in range(7)]
        PST = psum("PST", [128, 1024], BF16)
        bPST = Buf("PST")

        CM, CF = Cx(), Cx()
        CM.H, CM.bH = MIX, bMIX
        CM.fring = Ring("FM", NFM, lambda i: FRM[:, i, :])
        CM.bring = Ring("BM", NBM, lambda i: BRM[:, i, :])
        CM.pring = Ring("PM", 4, lambda i: PSB[i])
        CM.wring = Ring("WM", NWM, lambda i: i)
        CM.WR, CM.wname = WRM, "wm"
        CF.H, CF.bH = HF, bHF
        CF.fring = Ring("FF", NFF, lambda i: FRF[:, i, :])
        CF.bring = Ring("BF", NBF, lambda i: BRF[:, i, :])
        CF.pring = Ring("PF", 3, lambda i: PSB[4 + i])
        CF.wring = Ring("WF", NWF, lambda i: i)
        CF.WR, CF.wname = WRF, "wf"

        def ACT(out, in_, func, reads, writes, **kw):
            S.act(lambda e: e.activation(out=out, in_=in_, func=func, **kw), reads, writes)

        def MM(out, lhsT, rhs, start, stop, reads, writes):
            S.pe(lambda e: e.matmul(out, lhsT=lhsT, rhs=rhs, start=start, stop=stop), reads, writes)

        def STT(out, in0, scalar, in1, op0, op1, reads, writes):
            S.dve(lambda e: e.scalar_tensor_tensor(out=out, in0=in0, scalar=scalar, in1=in1, op0=op0, op1=op1),
                  reads, writes)

        def TT(eng, out, in0, in1, op, reads, writes):
            getattr(S, eng)(lambda e: e.tensor_tensor(out=out, in0=in0, in1=in1, op=op), reads, writes)

        def TS(eng, out, in0, s1, s2, op0, op1, reads, writes):
            if s2 is None:
                getattr(S, eng)(lambda e: e.tensor_scalar(out=out, in0=in0, scalar1=s1, scalar2=None, op0=op0),
                                reads, writes)
            else:
                getattr(S, eng)(lambda e: e.tensor_scalar(out=out, in0=in0, scalar1=s1, scalar2=s2, op0=op0, op1=op1),
                                reads, writes)

        def CP(eng, out, in_, reads, writes):
            if eng == "act":
                S.act(lambda e: e.activation(out=out, in_=in_, func=AF.Copy), reads, writes)
            else:
                getattr(S, eng)(lambda e: e.tensor_copy(out=out, in_=in_), reads, writes)

        def pcol(l, off, n=1):
            base = l * P_LAYER + off
            return PK[:, base:base + n]

        S.dma("sp", lambda e: e.dma_start(out=PK[:, :], in_=pk[:, :]), "pk", writes=[bPK])
        S.dma("sp", lambda e: e.dma_start(out=CN[:, :], in_=cn[:, :]), "cn", writes=[bCN])

        def load_x(t):
            ts_ = slice(t * N, (t + 1) * N)
            S.dma("sp", lambda e: e.dma_start(
                out=XTs[t % 2][:, :, :], in_=xT[:, ts_].rearrange("(k p) s -> p k s", p=128)),
                f"xl{t % 2}", writes=bXs[t % 2])

        load_x(0)
        load_x(1)
        for l in range(DEPTH):
            S.dma("pool", (lambda l: lambda e: e.dma_start(out=SM[:, l, :], in_=smat[l, :, :]))(l), "sm",
                  writes=[bSM], nodeps=True)
        bW16 = [[Buf(f"W16A_{l}", track=False), Buf(f"W16B_{l}", track=False)] for l in range(DEPTH)]
        for l in range(DEPTH):
            for s in range(NSLAB):
                o, wd = SLAB_OFF[s], SLAB_W[s]
                bb = 2048 if wd == 4096 else 1536
                grp = 0 if s < S_UP else 1
                S.dma("pool", (lambda l, o, wd, bb: lambda e: e.dma_start(
                    out=w16[l, :, o:o + wd].rearrange("p (a b) -> p a b", b=bb),
                    in_=w32[l, :, o:o + wd].rearrange("p (a b) -> p a b", b=bb)))(l, o, wd, bb),
                    f"cv{l}_{grp}", writes=[bW16[l][grp]], nodeps=True)
        S.dve(lambda e: e.memset(ONES[:, :], 1.0), writes=[bONES])
        S.dve(lambda e: e.tensor_copy(out=MASKB[:, :], in_=CN[:, C_MASK:C_MASK + 128]), reads=[bCN], writes=[bMASK])
        S.dve(lambda e: e.memset(IDF[:, :], 1.0), writes=[bIDF])
        S.pool(lambda e: e.affine_select(out=IDF[:, :], in_=IDF[:, :], pattern=[[1, 128]], base=0,
                                         channel_multiplier=-1, compare_op=ALU.is_equal, fill=0.0),
               reads=[bIDF], writes=[bIDF])
        S.pool(lambda e: e.tensor_copy(out=IDN[:, :], in_=IDF[:, :]), reads=[bIDF], writes=[bIDN])
        S.pool(lambda e: e.memset(KEZ[:, :, :], 0.0), writes=bKE)
        for (tl, bl) in ((Sf, bSf), (Sb, bSb)):
            S.pool((lambda tl: lambda e: e.memset(tl[:, :, :, :], 0.0))(tl), writes=[b for r in bl for b in r])
        S.pool(lambda e: e.memset(HL[:, :, :], 0.0), writes=[b for r in bHL for b in r])
        S.pool(lambda e: e.memset(UH[:, :, :, :], 0.0), writes=[b for r in bUH for b in r])
        S.pool(lambda e: e.memset(XH[:, :, :, :], 0.0), writes=[b for r in bXH for b in r])
        S.pool(lambda e: e.memset(FH[:, :, :, :], 0.0), writes=bFH)
        for l in range(DEPTH):
            TS("dve", DPK[:, l, 0:2], pcol(l, P_BG, 2), -1.0, None, ALU.mult, None, [bPK], [bDPK])
            fb, ft = CM.fring.get()
            ACT(ft[:, 0:2], pcol(l, P_LAM, 2), AF.Exp, [bPK], [fb], scale=-1.0)
            ACT(ft[:, 2:4], ft[:, 0:2], AF.Ln, [fb], [fb], bias=1.0)
            TS("dve", DPK[:, l, 2:4], ft[:, 2:4], -8.0, None, ALU.mult, None, [fb], [bDPK])

        def load_slab(cx, l, s):
            b, slot = cx.wring.get()
            o, wd = SLAB_OFF[s], SLAB_W[s]
            grp = 0 if s < S_UP else 1
            WRt = cx.WR
            S.dma("sp", lambda e: e.dma_start(out=WRt[:, slot, 0:wd], in_=w16[l, :, o:o + wd]),
                  f"{cx.wname}{slot}", reads=[bW16[l][grp]], writes=[b])
            return b, slot

        def rmsnorm(cx, X, bX):
            pb, pt = cx.pring.get()
            for k in range(8):
                qb, qt = cx.bring.get()
                ACT(qt, X[:, k, :], AF.Square, [bX[k]], [qb])
                MM(pt[:, :], ONES[:, :], qt, k == 0, k == 7, [bONES, qb], [pb])
            lb, lt = cx.fring.get()
            ACT(lt[:, 0:N], pt[:, :], AF.Ln, [pb], [lb], scale=1.0 / D, bias=EPS)
            rb, rt = cx.fring.get()
            ACT(rt[:, 0:N], lt[:, 0:N], AF.Exp, [lb], [rb], scale=-0.5)
            return rb, rt

        def norm_to_H(cx, X, bX, gcol_base):
            rb, rt = rmsnorm(cx, X, bX)
            for k in range(8):
                STT(cx.H[:, k, :], X[:, k, :], PK[:, gcol_base + k:gcol_base + k + 1], rt[:, 0:N],
                    ALU.mult, ALU.mult, [bX[k], bPK, rb], [cx.bH[k]])

        def norm_gen(cx, X, bX, gcol_base, ph):
            pb, pt = cx.pring.get()
            for k in range(8):
                qb, qt = cx.bring.get()
                ACT(qt, X[:, k, :], AF.Square, [bX[k]], [qb])
                MM(pt[:, :], ONES[:, :], qt, k == 0, k == 7, [bONES, qb], [pb])
                if k == 3:
                    yield ph
            yield ph
            lb, lt = cx.fring.get()
            ACT(lt[:, 0:N], pt[:, :], AF.Ln, [pb], [lb], scale=1.0 / D, bias=EPS)
            ACT(lt[:, 0:N], lt[:, 0:N], AF.Exp, [lb], [lb], scale=-0.5)
            yield ph
            for k in range(8):
                STT(cx.H[:, k, :], X[:, k, :], PK[:, gcol_base + k:gcol_base + k + 1], lt[:, 0:N],
                    ALU.mult, ALU.mult, [bX[k], bPK, lb], [cx.bH[k]])
                if k % 2 == 1:
                    yield ph

        def fm_group(cx, slot, wb, col0, M=128):
            pb, pt = cx.pring.get()
            for k in range(8):
                MM(pt[0:M, :], cx.WR[:, slot, k * 512 + col0:k * 512 + col0 + M], cx.H[:, k, :], k == 0, k == 7,
                   [wb, cx.bH[k]], [pb])
            return pb, pt

        pref = {}

        def mixer_gen(t, l, nxt=None):
            cx = CM
            X, bX = XTs[t % 2], bXs[t % 2]
            H, bH = cx.H, cx.bH
            fring, bring, pring = cx.fring, cx.bring, cx.pring
            first_tile = (t == 0)
            yield from norm_gen(cx, X, bX, l * P_LAYER + P_G1, 1)
            if ("M", t, l) in pref:
                wb, slot = pref.pop(("M", t, l))
            else:
                wb, slot = load_slab(cx, l, S_IN + 0)
            pb, pt = fm_group(cx, slot, wb, 256, M=16)
            gb_, GLOW = bring.get()
            ACT(GLOW[0:16, :], pt[0:16, :], AF.Copy, [pb], [gb_])
            yield 1
            cs = []
            for c in range(2):
                pb, pt = pring.get()
                MM(pt[:, :], SM[0:16, l, c * 128:(c + 1) * 128], GLOW[0:16, :], True, True, [bSM, gb_], [pb])
                eb_, et_ = fring.get()
                ACT(et_[:, 0:N], pt[:, :], AF.Exp, [pb, bDPK], [eb_], scale=-1.0, bias=DPK[:, l, c:c + 1])
                sb_, st_ = fring.get()
                ACT(st_[:, 0:N], et_[:, 0:N], AF.Ln, [eb_], [sb_], bias=1.0)
                cb_, ct_ = fring.get()
                S.dve((lambda ct_, st_: lambda e: e.tensor_tensor_scan(
                    out=ct_[:, 0:N], data0=CN[:, C_RESET:C_RESET + N], data1=st_[:, 0:N], initial=0.0,
                    op0=ALU.mult, op1=ALU.add))(ct_, st_), [sb_, bCN], [cb_])
                cs.append((cb_, ct_))
                yield 1
            ebs, enbs = [], []
            for c in range(2):
                cb_, ct_ = cs[c]
                b1, t1 = fring.get()
                ACT(t1[:, 0:N], ct_[:, 0:N], AF.Exp, [cb_], [b1], scale=-1.0 / 16)
                b2, t2 = fring.get()
                ACT(t2[:, 0:N], ct_[:, 0:N], AF.Exp, [cb_], [b2], scale=1.0 / 16)
                ACT(Dd[:, c, :], ct_[:, 127:N:128], AF.Exp, [cb_], [bDd[c]], scale=-1.0 / 16)
                ebs.append((b1, t1))
                enbs.append((b2, t2))
            for c in range(2):
                pb, pt = fm_group(cx, slot, wb, c * 128)
                ACT(GY[:, c, :], pt[:, :], AF.Gelu_apprx_tanh, [pb], [bGY[c]])
                yield 1
            wb, slot = load_slab(cx, l, S_IN + 1)
            for c in range(2):
                pb, pt = fm_group(cx, slot, wb, c * 128)
                STT(QE[:, c, :], pt[:, :], 0.125, ebs[c][1][:, 0:N], ALU.mult, ALU.mult, [pb, ebs[c][0]], [bQE[c]])
                yield 1
            for c in range(2):
                pb, pt = fm_group(cx, slot, wb, 256 + c * 128)
                ent = enbs[c][1]
                for a in range(2):
                    ps_ = slice(a * 64, (a + 1) * 64)
                    TT("dve", KEZ[ps_, 2 * c + a, :], pt[ps_, :], ent[ps_, 0:N], ALU.mult, [pb, enbs[c][0]], [bKE[c]])
                db_, dt_ = fring.get()
                for blk in range(4):
                    TS("dve", dt_[:, blk * 128:(blk + 1) * 128], ent[:, blk * 128:(blk + 1) * 128],
                       Dd[:, c, blk:blk + 1], None, ALU.mult, None, [enbs[c][0], bDd[c]], [db_])
                kb_, KDT = bring.get()
                TT("dve", KDT, pt[:, :], dt_[:, 0:N], ALU.mult, [pb, db_], [kb_])
                yield 1
                for blk in range(4):
                    S.pe((lambda KDT, blk: lambda e: e.transpose(
                        PST[:, blk * 128:(blk + 1) * 128], KDT[:, blk * 128:(blk + 1) * 128], IDN[:, :]))(KDT, blk),
                        [kb_, bIDN], [bPST])
                CP("act", KD_TM[:, c, :], PST[:, 0:N], [bPST], [bKD[c]])
                yield 1
            wb, slot = load_slab(cx, l, S_IN + 2)
            for blk in range(4):
                pb, pt = pring.get()
                for k in range(8):
                    MM(pt[:, :], H[:, k, blk * 128:(blk + 1) * 128], cx.WR[:, slot, k * 512:(k + 1) * 512],
                       k == 0, k == 7, [wb, bH[k]], [pb])
                CP("act", V_TM[:, blk, :], pt[:, :], [pb], [bV[blk]])
                yield 1
            wb, slot = load_slab(cx, l, S_IN + 3)
            for h in range(4):
                pb, pt = fm_group(cx, slot, wb, h * 128)
                fb, ft = fring.get()
                ACT(ft[:, 0:N], pt[:, :], AF.Silu, [pb], [fb])
                TS("dve", SILU[:, h, :], ft[:, 0:N], pcol(l, P_GN + h), None, ALU.mult, None, [fb, bPK], [bSILU[h]])
                yield 1
            wb, slot = load_slab(cx, l, S_IN + 4)
            for c in range(2):
                CP("pool", U[:, c, 0:16], UH[:, l, c, :], [bUH[l][c]], [bU[c]])
                pb, pt = fm_group(cx, slot, wb, c * 128)
                CP("act", U[:, c, 16:528], pt[:, :], [pb], [bU[c]])
                CP("pool", UH[:, l, c, :], U[:, c, 512:528], [bU[c]], [bUH[l][c]])
                yield 1
            for c in range(2):
                CP("pool", XR[:, c, 0:3], XH[:, l, c, 0:3], [bXH[l][c]], [bXR[c]])
                pb, pt = fm_group(cx, slot, wb, 256 + c * 128)
                CP("act", XR[:, c, 3:515], pt[:, :], [pb], [bXR[c]])
                CP("pool", XH[:, l, c, 0:3], XR[:, c, 512:515], [bXR[c]], [bXH[l][c]])
                yield 1

            psrc = []
            for c in range(2):
                u = U[:, c, :]
                b2, t2 = fring.get()
                TT("pool", t2[:, 1:528], u[:, 1:528], u[:, 0:527], ALU.add, [bU[c]], [b2])
                b4, t4 = fring.get()
                TT("pool", t4[:, 3:528], t2[:, 3:528], t2[:, 1:526], ALU.add, [b2], [b4])
                if c == 0:
                    psrc.append(((b2, t2, 2, 0), (b4, t4, 4, 1)))
                else:
                    b8, t8 = fring.get()
                    TT("pool", t8[:, 7:528], t4[:, 7:528], t4[:, 3:524], ALU.add, [b4], [b8])
                    b16, t16 = fring.get()
                    TT("pool", t16[:, 15:528], t8[:, 15:528], t8[:, 7:520], ALU.add, [b8], [b16])
                    psrc.append(((b8, t8, 8, 2), (b16, t16, 16, 3)))
            xcs = []
            for c in range(2):
                xb_, xc = fring.get()
                TS("pool", xc[:, 0:N], XR[:, c, 3:515], pcol(l, P_LCW + c * 4 + 3), pcol(l, P_LCB + c),
                   ALU.mult, ALU.add, [bXR[c], bPK], [xb_])
                xcs.append((xb_, xc))
            yield 2
            yield 2
            yield 2
            dpls = []
            if first_tile:
                fb, ft = fring.get()
            for c in range(2):
                u = U[:, c, :]
                dpb, DPLc = bring.get()
                for a, (sbuf_, stile, win, wi) in enumerate(psrc[c]):
                    ps_ = slice(a * 64, (a + 1) * 64)
                    STT(DPLc[ps_, :], stile[ps_, 16:528], 1.0 / win, u[ps_, 16:528], ALU.mult, ALU.subtract,
                        [sbuf_, bU[c]], [dpb])
                    if first_tile:
                        fcol = slice(c * 16, (c + 1) * 16)
                        TT("dve", ft[ps_, fcol], stile[ps_, 16:32], CN[ps_, C_INV + wi * 16:C_INV + (wi + 1) * 16],
                           ALU.mult, [sbuf_, bCN], [fb])
                        TT("dve", DPLc[ps_, 0:16], ft[ps_, fcol], u[ps_, 16:32], ALU.subtract,
                           [fb, bU[c], dpb], [dpb])
                dpls.append((dpb, DPLc))
            for c in range(2):
                xb_, xc = xcs[c]
                for k in range(3):
                    STT(xc[:, 0:N], XR[:, c, k:k + N], pcol(l, P_LCW + c * 4 + k), xc[:, 0:N], ALU.mult, ALU.add,
                        [bXR[c], bPK, xb_], [xb_])
            yield 2
            yield 2
            cbs = []
            for c in range(2):
                cbb, cbt = bring.get()
                CP("pool", cbt, xcs[c][1][:, 0:N], [xcs[c][0]], [cbb])
                cbs.append((cbb, cbt))
            pps = []
            for c in range(2):
                pb, pt = pring.get()
                MM(pt[:, :], SM[:, l, 256 + c * 128:256 + (c + 1) * 128], dpls[c][1], True, True, [bSM, dpls[c][0]], [pb])
                pps.append((pb, pt))
            yield 2
            yield 2
            for c in range(2):
                pb, pt = pps[c]
                ACT(MIX[:, 4 + c, :], pt[:, :], AF.Identity, [pb, bPK], [bMIX[4 + c]], scale=pcol(l, P_PSC + c))
            lr = []
            for c in range(2):
                cbb, cbt = cbs[c]
                pa, pat = pring.get()
                MM(pat[:, :], SM[:, l, 512 + c * 128:512 + (c + 1) * 128], cbt, True, True, [bSM, cbb], [pa])
                pi, pit = pring.get()
                MM(pit[:, :], SM[:, l, 768 + c * 128:768 + (c + 1) * 128], cbt, True, True, [bSM, cbb], [pi])
                yield 2
                rb_, rt_ = fring.get()
                ACT(rt_[:, 0:N], pat[:, :], AF.Sigmoid, [pa, bPK], [rb_], bias=pcol(l, P_LBA + c))
                ib_, it_ = fring.get()
                ACT(it_[:, 0:N], pit[:, :], AF.Sigmoid, [pi, bPK], [ib_], bias=pcol(l, P_LBX + c))
                lr.append((rb_, rt_, ib_, it_))
            yield 2
            for c in range(2):
                rb_, rt_, ib_, it_ = lr[c]
                ACT(rt_[:, 0:N], rt_[:, 0:N], AF.Exp, [rb_, bDPK], [rb_], scale=DPK[:, l, 2 + c:3 + c])
                TT("pool", it_[:, 0:N], it_[:, 0:N], xcs[c][1][:, 0:N], ALU.mult, [ib_, xcs[c][0]], [ib_])
            yield 2
            qs = []
            for c in range(2):
                rb_, rt_, ib_, it_ = lr[c]
                a2b, a2t = fring.get()
                TT("dve", a2t[:, 0:N], rt_[:, 0:N], rt_[:, 0:N], ALU.mult, [rb_], [a2b])
                qs.append((a2b, a2t))
            yield 2
            for c in range(2):
                a2b, a2t = qs[c]
                ACT(a2t[:, 0:N], a2t[:, 0:N], AF.Sqrt, [a2b], [a2b], scale=-1.0, bias=1.0)
            yield 2
            yield 2
            for c in range(2):
                rb_, rt_, ib_, it_ = lr[c]
                a2b, a2t = qs[c]
                xb_, xc = xcs[c]
                TT("dve", a2t[:, 0:N], a2t[:, 0:N], it_[:, 0:N], ALU.mult, [a2b, ib_], [a2b])
                S.dve((lambda xc, rt_, a2t, c: lambda e: e.tensor_tensor_scan(
                    out=xc[:, 0:N], data0=rt_[:, 0:N], data1=a2t[:, 0:N], initial=HL[:, l, c:c + 1],
                    op0=ALU.mult, op1=ALU.add))(xc, rt_, a2t, c), [rb_, a2b, bHL[l][c]], [xb_])
                CP("dve", HL[:, l, c:c + 1], xc[:, N - 1:N], [xb_], [bHL[l][c]])
                TT("dve", MIX[:, 6 + c, :], xc[:, 0:N], GY[:, c, :], ALU.mult, [xb_, bGY[c]], [bMIX[6 + c]])
            yield 2

            g_sc, g_kv, g_st, g_ot, g_q, g_pre, g_ss, g_rt = {}, {}, {}, {}, {}, {}, {}, {}

            def gla_stage(s, blk):
                bs = slice(blk * 128, (blk + 1) * 128)
                if s == 0:
                    pb, pt = pring.get()
                    for h in range(4):
                        hp, a = divmod(h, 2)
                        MM(pt[:, h * 128:(h + 1) * 128], KEZ[:, h, bs], QE[:, hp, bs], True, True,
                           [bKE[hp], bQE[hp]], [pb])
                    g_sc[blk] = (pb, pt)
                    kb, kt = pring.get()
                    for hp in range(2):
                        MM(kt[:, hp * 256:(hp + 1) * 256], KD_TM[:, hp, bs], V_TM[:, blk, hp * 256:(hp + 1) * 256],
                           True, True, [bKD[hp], bV[blk]], [kb])
                    g_kv[blk] = (kb, kt)
                elif s == 1:
                    pb, pt = g_sc[blk]
                    sb_, st_ = scring.get()
                    TT("dve", st_.rearrange("p (h n) -> p h n", h=4), pt[:, :].rearrange("p (h n) -> p h n", h=4),
                       MASKB[:, :].unsqueeze(1).to_broadcast([128, 4, 128]), ALU.mult, [pb, bMASK], [sb_])
                    g_st[blk] = (sb_, st_)
                    kb, kt = g_kv[blk]
                    for hp in range(2):
                        for a in range(2):
                            ps_ = slice(a * 64, (a + 1) * 64)
                            STT(Sf[ps_, l, hp, :], Sf[ps_, l, hp, :], Dd[ps_, hp, blk:blk + 1],
                                kt[ps_, hp * 256 + a * 128:hp * 256 + (a + 1) * 128], ALU.mult, ALU.add,
                                [bSf[l][hp], bDd[hp], kb], [bSf[l][hp]])
                elif s == 2:
                    sb_, st_ = g_st[blk]
                    ob, ot = pring.get()
                    for h in range(4):
                        hp, a = divmod(h, 2)
                        MM(ot[:, h * 128:(h + 1) * 128], V_TM[:, blk, h * 128:(h + 1) * 128],
                           st_[:, h * 128:(h + 1) * 128], True, False, [bV[blk], sb_], [ob])
                        MM(ot[:, h * 128:(h + 1) * 128], Sb[:, l, h, :], QE[:, hp, bs], False, True,
                           [bSb[l][hp], bQE[hp]], [ob])
                    g_ot[blk] = (ob, ot)
                    for hp in range(2):
                        for a in range(2):
                            ps_ = slice(a * 64, (a + 1) * 64)
                            CP("act", Sb[ps_, l, 2 * hp + a, :], Sf[ps_, l, hp, :], [bSf[l][hp]], [bSb[l][hp]])
                elif s == 3:
                    ob, ot = g_ot[blk]
                    qb, qt = bring.get()
                    ACT(qt, ot[:, :], AF.Square, [ob], [qb])
                    g_q[blk] = (qb, qt)
                    tb, tt = fring.get()
                    TT("dve", tt[:, 0:N].rearrange("p (h n) -> p h n", h=4),
                       ot[:, :].rearrange("p (h n) -> p h n", h=4), SILU[:, :, bs], ALU.mult, [ob] + bSILU, [tb])
                    g_pre[blk] = (tb, tt)
                elif s == 4:
                    qb, qt = g_q[blk]
                    nb_, nt_ = pring.get()
                    MM(nt_[:, :], ONES[:, :], qt, True, True, [bONES, qb], [nb_])
                    g_ss[blk] = (nb_, nt_)
                elif s == 5:
                    nb_, nt_ = g_ss[blk]
                    lb, lt = fring.get()
                    ACT(lt[:, 0:N], nt_[:, :], AF.Ln, [nb_], [lb], scale=1.0 / 128, bias=EPS)
                    ACT(lt[:, 0:N], lt[:, 0:N], AF.Exp, [lb], [lb], scale=-0.5)
                    g_rt[blk] = (lb, lt)
                elif s == 6:
                    tb, tt = g_pre[blk]
                    lb, lt = g_rt[blk]
                    TT("dve", MIX[:, 0:4, bs], tt[:, 0:N].rearrange("p (h n) -> p h n", h=4),
                       lt[:, 0:N].rearrange("p (h n) -> p h n", h=4), ALU.mult, [tb, lb], bMIX[0:4])

            for slot in range(2 * 3 + 7):
                for s in range(6, -1, -1):
                    if (slot - s) % 2 == 0 and 0 <= (slot - s) // 2 < 4:
                        gla_stage(s, (slot - s) // 2)
                yield 2

            slabs = [load_slab(cx, l, S_OUT + i) for i in range(2)]
            for n in range(8):
                if n == 4 and nxt is not None:
                    pref[("M",) + nxt] = load_slab(cx, nxt[1], S_IN + 0)
                wb, slot = slabs[n // 4]
                col0 = (n % 4) * 128
                pb, pt = pring.get()
                for k in range(8):
                    MM(pt[:, :], cx.WR[:, slot, k * 512 + col0:k * 512 + col0 + 128], MIX[:, k, :], k == 0, k == 7,
                       [wb, bMIX[k]], [pb])
                TT("dve", X[:, n, :], X[:, n, :], pt[:, :], ALU.add, [bX[n], pb], [bX[n]])
                yield 3

        def ffn_pre(t, l):
            cx = CF
            X, bX = XTs[t % 2], bXs[t % 2]
            yield from norm_gen(cx, X, bX, l * P_LAYER + P_G2, 4)
            fcw = PK[:, l * P_LAYER + P_FCW:l * P_LAYER + P_FCW + 144].rearrange("p (g k) -> p g k", k=3)
            TT("dve", CORR[:, :, 1], FH[:, l, :, 1], fcw[:, :, 0], ALU.mult, [bFH[l], bPK], [bCORR])
            TT("dve", CORR[:, :, 0], FH[:, l, :, 1], fcw[:, :, 1], ALU.mult, [bFH[l], bPK], [bCORR])
            TT("dve", CTMP[:, :], FH[:, l, :, 0], fcw[:, :, 0], ALU.mult, [bFH[l], bPK], [bCTMP])
            TT("dve", CORR[:, :, 0], CORR[:, :, 0], CTMP[:, :], ALU.add, [bCORR, bCTMP], [bCORR])
            yield 4

        def ffn_gen(t, l, nxt=None):
            cx = CF
            X, bX = XTs[t % 2], bXs[t % 2]
            fring, bring, pring = cx.fring, cx.bring, cx.pring
            if not EARLY_PRE:
                yield from ffn_pre(t, l)

            def conv_group(slot, wb, col0, g):
                pb, pt = fm_group(cx, slot, wb, col0)
                ab_, at_ = fring.get()
                base = l * P_LAYER + P_FCW + g * 3
                ACT(at_[:, 0:N], pt[:, :], AF.Identity, [pb, bPK], [ab_], scale=PK[:, base + 2:base + 3],
                    bias=pcol(l, P_FCB + g))
                STT(at_[:, 1:N], pt[:, 0:N - 1], PK[:, base + 1:base + 2], at_[:, 1:N], ALU.mult, ALU.add,
                    [pb, bPK, ab_], [ab_])
                STT(at_[:, 2:N], pt[:, 0:N - 2], PK[:, base:base + 1], at_[:, 2:N], ALU.mult, ALU.add,
                    [pb, bPK, ab_], [ab_])
                TT("pool", at_[:, 0:2], at_[:, 0:2], CORR[:, g, :], ALU.add, [ab_, bCORR], [ab_])
                CP("act", FH[:, l, g, :], pt[:, N - 2:N], [pb], [bFH[l]])
                return ab_, at_

            def gating(fch, gb, gt, vb, vt):
                ggb, ggt = bring.get()
                ACT(ggt, gt[:, 0:N], AF.Gelu_apprx_tanh, [gb], [ggb])
                TT("pool", A24[:, fch, :], ggt, vt[:, 0:N], ALU.mult, [ggb, vb], [bA24[fch]])

            pend = None
            for i in range(6):
                if i == 0 and ("F", t, l) in pref:
                    (wg, sg), (wv, sv) = pref.pop(("F", t, l))
                else:
                    wg, sg = load_slab(cx, l, S_UP + 2 * i)
                    wv, sv = load_slab(cx, l, S_UP + 2 * i + 1)
                for j in range(4):
                    fch = 4 * i + j
                    gb, gt = conv_group(sg, wg, j * 128, fch)
                    yield 8
                    vb, vt = conv_group(sv, wv, j * 128, 24 + fch)
                    if pend is not None:
                        gating(*pend)
                    pend = (fch, gb, gt, vb, vt)
                    yield 8
            gating(*pend)
            for n in range(8):
                wb, slot = load_slab(cx, l, S_DN + n)
                if n == 7 and nxt is not None:
                    pref[("F",) + nxt] = [load_slab(cx, nxt[1], S_UP + 0), load_slab(cx, nxt[1], S_UP + 1)]
                pb, pt = pring.get()
                for j in range(24):
                    MM(pt[:, :], cx.WR[:, slot, j * 128:(j + 1) * 128], A24[:, j, :], j == 0, j == 23,
                       [wb, bA24[j]], [pb])
                    if j % 8 == 7:
                        yield 8
                TT("dve", X[:, n, :], X[:, n, :], pt[:, :], ALU.add, [bX[n], pb], [bX[n]])
            if l == DEPTH - 1:
                rb, rt = rmsnorm(cx, X, bX)
                gb = DEPTH * P_LAYER
                for k in range(8):
                    STT(X[:, k, :], X[:, k, :], PK[:, gb + k:gb + k + 1], rt[:, 0:N], ALU.mult, ALU.mult,
                        [bX[k], bPK, rb], [bX[k]])
                ts_ = slice(t * N, (t + 1) * N)
                S.dma("act", lambda e: e.dma_start(
                    out=outT[:, ts_].rearrange("(k p) s -> p k s", p=128), in_=X[:, :, :]), f"st{t % 2}", reads=bX)
                if t + 2 < NT:
                    load_x(t + 2)
                yield 8

        TL = [(t, l) for pair in range(NT // 2) for l in range(DEPTH) for t in (2 * pair, 2 * pair + 1)]
        def mside(t, l, nxt=None):
            yield from mixer_gen(t, l, nxt)
            if EARLY_PRE:
                yield from ffn_pre(t, l)

        S.dry = True
        cnt = {1: 0, 2: 0, 3: 0, 4: 0}
        for ph in mside(2, 0):
            cnt[ph] += 1
        S.dry = False
        xs = [0]
        for ph in ((1, 2, 3, 4) if EARLY_PRE else (1, 2, 3)):
            xs.append(xs[-1] + cnt[ph])
        ys = M_SCHED_Y if EARLY_PRE else M_SCHED_Y3

        def m_target(i):
            for j in range(1, len(xs)):
                if i <= xs[j]:
                    return ys[j - 1] + (ys[j] - ys[j - 1]) * (i - xs[j - 1]) / float(xs[j] - xs[j - 1])
            return ys[-1]

        for step in range(len(TL) + 1):
            nxt_m = TL[step + 1] if step + 1 < len(TL) else None
            nxt_f = TL[step] if step < len(TL) else None
            gm = mside(*TL[step], nxt=nxt_m) if step < len(TL) else None
            gf = ffn_gen(*TL[step - 1], nxt=nxt_f) if step >= 1 else None
            pm = 0
            pf = 0.0
            while gm is not None or gf is not None:
                run_m = gm is not None and (gf is None or m_target(pm) <= pf / FFN_W)
                if run_m:
                    try:
                        next(gm)
                        pm += 1
                    except StopIteration:
                        gm = None
                else:
                    try:
                        pf += next(gf)
                    except StopIteration:
                        gf = None
        global _SBUF_LEFT
        _SBUF_LEFT = nc.sbuf_bytes_remaining
        S.emit(final_waits=["st0", "st1"])
    return nc


MIXER_W = 54.0
EARLY_PRE = False
M_SCHED_Y = (0.02, 0.36, 0.80, 0.86, 0.95)
M_SCHED_Y3 = (0.0, 0.50, 0.96, 1.0)
FFN_W = 8.0 * (48 + 24)


def _slab_k(Wc):
    return np.ascontiguousarray(Wc.reshape(8, 128, 512).transpose(1, 0, 2)).reshape(128, 4096)


def _fm(v, n):
    return np.ascontiguousarray(v.reshape(n, 128).T)


def prep_host(inp, DEPTH):
    w32 = np.zeros((DEPTH, 128, WCOLS), np.float32)
    smat = np.zeros((DEPTH, 128, 1024), np.float32)
    pk = np.zeros((128, DEPTH * P_LAYER + 8), np.float32)
    for l in range(DEPTH):
        wi = inp["w_in"][l]
        q, k, v, g = wi[:, 0:256], wi[:, 256:512], wi[:, 512:1024], wi[:, 1024:1536]
        glow, pu, lx, ly = wi[:, 1536:1552], wi[:, 1552:1808], wi[:, 1808:2064], wi[:, 2064:2320]
        z = np.zeros((1024, 240), np.float32)
        slabs = [np.concatenate([ly, glow, z], 1), np.concatenate([q, k], 1), v, g, np.concatenate([pu, lx], 1)]
        wo = inp["w_out"][l]
        slabs += [wo[:, 0:512], wo[:, 512:1024]]
        wu = inp["ffn_w_up"][l]
        for i in range(6):
            slabs += [wu[:, 512 * i:512 * (i + 1)], wu[:, 3072 + 512 * i:3072 + 512 * (i + 1)]]
        for s, sl in enumerate(slabs):
            w32[l, :, SLAB_OFF[s]:SLAB_OFF[s + 1]] = _slab_k(sl)
        wd = inp["ffn_w_down"][l]
        for n in range(8):
            blk = wd[:, n * 128:(n + 1) * 128].reshape(24, 128, 128).transpose(1, 0, 2).reshape(128, 3072)
            s = S_DN + n
            w32[l, :, SLAB_OFF[s]:SLAB_OFF[s + 1]] = blk
        smat[l, 0:16, 0:256] = inp["gla_wg2"][l]
        for c in range(2):
            for a in range(2):
                r = slice(a * 64, (a + 1) * 64)
                smat[l, r, 256 + c * 128 + a * 64:256 + c * 128 + (a + 1) * 64] = inp["pool_w"][l, 2 * c + a]
                smat[l, r, 512 + c * 128 + a * 64:512 + c * 128 + (a + 1) * 64] = inp["lru_wa"][l, 2 * c + a]
                smat[l, r, 768 + c * 128 + a * 64:768 + c * 128 + (a + 1) * 64] = inp["lru_wx"][l, 2 * c + a]
        b = l * P_LAYER
        pk[:, b + P_G1:b + P_G1 + 8] = _fm(inp["norm1_g"][l], 8)
        pk[:, b + P_G2:b + P_G2 + 8] = _fm(inp["norm2_g"][l], 8)
        pk[:, b + P_BG:b + P_BG + 2] = _fm(inp["gla_bg"][l], 2)
        pk[:, b + P_GN:b + P_GN + 4] = inp["gla_norm_g"][l].T
        pk[:, b + P_PSC:b + P_PSC + 2] = _fm(inp["pool_scale"][l], 2)
        for c in range(2):
            pk[:, b + P_LCW + c * 4:b + P_LCW + c * 4 + 4] = inp["lru_conv_w"][l][:, c * 128:(c + 1) * 128].T
        pk[:, b + P_LCB:b + P_LCB + 2] = _fm(inp["lru_conv_b"][l], 2)
        pk[:, b + P_LBA:b + P_LBA + 2] = _fm(inp["lru_ba"][l], 2)
        pk[:, b + P_LBX:b + P_LBX + 2] = _fm(inp["lru_bx"][l], 2)
        pk[:, b + P_LAM:b + P_LAM + 2] = _fm(inp["lru_lambda"][l], 2)
        fw = inp["ffn_conv_w"][l]
        pk[:, b + P_FCW:b + P_FCW + 144] = fw.reshape(3, 48, 128).transpose(2, 1, 0).reshape(128, 144)
        pk[:, b + P_FCB:b + P_FCB + 48] = _fm(inp["ffn_conv_b"][l], 48)
    pk[:, DEPTH * P_LAYER:DEPTH * P_LAYER + 8] = _fm(inp["final_g"], 8)
    cn = np.zeros((128, NCN), np.float32)
    j = np.arange(128)[:, None]
    i = np.arange(128)[None, :]
    cn[:, C_MASK:C_MASK + 128] = (j <= i)
    rs = np.ones(512, np.float32)
    rs[::128] = 0.0
    cn[:, C_RESET:C_RESET + 512] = rs[None, :]
    for wi_, win in enumerate((2, 4, 8, 16)):
        cn[:, C_INV + wi_ * 16:C_INV + (wi_ + 1) * 16] = (1.0 / np.minimum(np.arange(1, 17), win))[None, :]
    return w32, smat, pk, cn


_NC_CACHE = {}


def run(inp, S_len, DEPTH, n_cores=8):
    key = (S_len, DEPTH)
    if key not in _NC_CACHE:
        _NC_CACHE[key] = build_nc(S_len, DEPTH)
    nc = _NC_CACHE[key]
    w32, smat, pk, cn = prep_host(inp, DEPTH)
    x = inp["x"]
    in_maps = []
    for b in range(n_cores):
        in_maps.append({"xT": np.ascontiguousarray(x[b].T), "w32": w32, "smat": smat, "pk": pk, "cn": cn})
    res = run_bass_kernel_spmd(nc, in_maps, core_ids=list(range(n_cores)))
    out = np.stack([np.ascontiguousarray(res.results[b]["outT"].T) for b in range(n_cores)], 0)
    return out.astype(np.float32)


def kernel(**inputs):
    inp = {k: np.asarray(v) for k, v in inputs.items()}
    return run(inp, 4096, 4, 8)
```

```python
import numpy as np
from contextlib import ExitStack
import concourse.bass as bass
import concourse.mybir as mybir
from concourse.bass_utils import run_bass_kernel_spmd

F32 = mybir.dt.float32
BF16 = mybir.dt.bfloat16
AF = mybir.ActivationFunctionType
ALU = mybir.AluOpType

D = 1024
NT_TOK = 512
EPS = 1e-6
N_IN_SLABS, N_OUT_SLABS, N_UP_SLABS, N_DN_SLABS = 5, 2, 12, 8
SLAB_W = [4096] * (N_IN_SLABS + N_OUT_SLABS + N_UP_SLABS) + [3072] * N_DN_SLABS
SLAB_OFF = [0]
for _w in SLAB_W:
    SLAB_OFF.append(SLAB_OFF[-1] + _w)
WCOLS = SLAB_OFF[-1]
NSLAB = len(SLAB_W)
S_IN, S_OUT, S_UP, S_DN = 0, N_IN_SLABS, N_IN_SLABS + N_OUT_SLABS, N_IN_SLABS + N_OUT_SLABS + N_UP_SLABS

P_G1, P_G2, P_BG, P_GN, P_PSC, P_LCW, P_LCB, P_LBA, P_LBX, P_LAM, P_FCW, P_FCB = (
    0, 8, 16, 18, 22, 24, 32, 34, 36, 38, 40, 184)
P_LAYER = 232
C_MASK, C_RESET, C_INV = 0, 128, 640
NCN = 704

ENGS = ("pe", "act", "dve", "pool", "sp")


class Buf:
    __slots__ = ("name", "w", "r", "track")

    def __init__(self, name, track=True):
        self.name = name
        self.w = None
        self.r = {}
        self.track = track


class Sched:
    def __init__(self, nc):
        self.nc = nc
        self.ops = {e: [] for e in ENGS}
        self.dma_sems = {}
        self.dry = False

    def _add(self, eng, fn, reads, writes, dma_sem=None, nodeps=False):
        if self.dry:
            return None
        idx = len(self.ops[eng])
        keep = []
        if not nodeps:
            deps = []
            for b in reads:
                if b.w is not None:
                    deps.append((b.w, "raw"))
            for b in writes:
                if b.w is not None:
                    deps.append((b.w, "waw"))
                for tk in b.r.values():
                    deps.append((tk, "war"))
            for tk, kind in deps:
                if tk[0] == "E" and tk[1] == eng and dma_sem is None and eng == "pe":
                    continue
                keep.append(tk)
        if dma_sem is None:
            tok = ("E", eng, idx)
        else:
            ent = self.dma_sems.setdefault(dma_sem, [None, 0])
            ent[1] += 16
            tok = ("D", dma_sem, ent[1])
        for b in reads:
            if b.track:
                old = b.r.get(tok[1])
                if old is None or old[2] < tok[2]:
                    b.r[tok[1]] = tok
        for b in writes:
            b.w = tok
            b.r = {}
        self.ops[eng].append({"fn": fn, "deps": keep, "dma": dma_sem})
        return tok

    def pe(self, fn, reads=(), writes=()):
        return self._add("pe", fn, reads, writes)

    def act(self, fn, reads=(), writes=()):
        return self._add("act", fn, reads, writes)

    def dve(self, fn, reads=(), writes=()):
        return self._add("dve", fn, reads, writes)

    def pool(self, fn, reads=(), writes=()):
        return self._add("pool", fn, reads, writes)

    def dma(self, queue, fn, sem, reads=(), writes=(), nodeps=False):
        return self._add(queue, fn, reads, writes, dma_sem=sem, nodeps=nodeps)

    def emit(self, final_waits=()):
        nc = self.nc
        signaled = {e: set() for e in ENGS}
        for e in ENGS:
            for op in self.ops[e]:
                for tk in op["deps"]:
                    if tk[0] == "E":
                        signaled[tk[1]].add(tk[2])
        cum = {}
        for e in ENGS:
            c = 0
            m = {}
            for i in range(len(self.ops[e])):
                if i in signaled[e]:
                    c += 1
                    m[i] = c
            cum[e] = m
        with ExitStack() as es:
            esem = {e: es.enter_context(nc.semaphore("s_" + e)) for e in ENGS}
            for name, ent in self.dma_sems.items():
                ent[0] = es.enter_context(nc.semaphore("d_" + name))
            block = es.enter_context(nc.Block())

            def run(ename, eng):
                waited = {}
                for i, op in enumerate(self.ops[ename]):
                    need = {}
                    for tk in op["deps"]:
                        if tk[0] == "E":
                            key = ("E", tk[1])
                            val = cum[tk[1]][tk[2]]
                        else:
                            key = ("D", tk[1])
                            val = tk[2]
                        if need.get(key, 0) < val:
                            need[key] = val
                    for key, val in need.items():
                        if waited.get(key, 0) >= val:
                            continue
                        waited[key] = val
                        sem = esem[key[1]] if key[0] == "E" else self.dma_sems[key[1]][0]
                        eng.wait_ge(sem, val)
                    ins = op["fn"](eng)
                    if op["dma"] is not None:
                        ins.then_inc(self.dma_sems[op["dma"]][0], 16)
                    elif i in signaled[ename]:
                        ins.then_inc(esem[ename], 1)
                if ename == "act":
                    for name in final_waits:
                        ent = self.dma_sems[name]
                        eng.wait_ge(ent[0], ent[1])

            @block.sync
            def _(eng):
                run("sp", eng)

            @block.tensor
            def _(eng):
                run("pe", eng)

            @block.scalar
            def _(eng):
                run("act", eng)

            @block.vector
            def _(eng):
                run("dve", eng)

            @block.gpsimd
            def _(eng):
                run("pool", eng)


class Ring:
    def __init__(self, name, n, apf):
        self.bufs = [Buf(f"{name}{i}") for i in range(n)]
        self.apf = apf
        self.n = n
        self.i = 0

    def get(self):
        i = self.i
        self.i = (i + 1) % self.n
        return self.bufs[i], self.apf(i)


class Cx:
    pass


def build_nc(S_len, DEPTH):
    NT = S_len // NT_TOK
    assert NT >= 2 and NT % 2 == 0
    N = NT_TOK
    NPK = DEPTH * P_LAYER + 8
    nc = bass.Bass("TRN2", target_bir_lowering=False)
    xT = nc.dram_tensor("xT", [D, S_len], F32, kind="ExternalInput").ap()
    w32 = nc.dram_tensor("w32", [DEPTH, 128, WCOLS], F32, kind="ExternalInput").ap()
    smat = nc.dram_tensor("smat", [DEPTH, 128, 1024], F32, kind="ExternalInput").ap()
    pk = nc.dram_tensor("pk", [128, NPK], F32, kind="ExternalInput").ap()
    cn = nc.dram_tensor("cn", [128, NCN], F32, kind="ExternalInput").ap()
    outT = nc.dram_tensor("outT", [D, S_len], F32, kind="ExternalOutput").ap()
    w16 = nc.dram_tensor("w16", [DEPTH, 128, WCOLS], BF16, kind="Internal").ap()
    S = Sched(nc)

    with ExitStack() as es:
        def sb(name, shape, dt):
            return es.enter_context(nc.sbuf_tensor(name, shape, dt))

        def psum(name, shape, dt):
            return es.enter_context(nc.psum_tensor(name, shape, dt))

        XTs = [sb(f"XT{i}", [128, 8, N], F32) for i in range(2)]
        bXs = [[Buf(f"X{i}_{k}") for k in range(8)] for i in range(2)]
        MIX = sb("MIX", [128, 8, N], BF16)
        bMIX = [Buf(f"MIX{k}") for k in range(8)]
        HF = sb("HF", [128, 8, N], BF16)
        bHF = [Buf(f"HF{k}") for k in range(8)]
        A24 = sb("A24", [128, 24, N], BF16)
        bA24 = [Buf(f"A24_{j}") for j in range(24)]
        PK = sb("PK", [128, NPK], F32)
        bPK = Buf("PK", track=False)
        DPK = sb("DPK", [128, DEPTH, 4], F32)
        bDPK = Buf("DPK", track=False)
        SM = sb("SM", [128, DEPTH, 1024], BF16)
        bSM = Buf("SM", track=False)
        CN = sb("CN", [128, NCN], F32)
        bCN = Buf("CN", track=False)
        MASKB = sb("MASKB", [128, 128], BF16)
        ONES = sb("ONES", [128, 128], BF16)
        IDN = sb("IDN", [128, 128], BF16)
        IDF = sb("IDF", [128, 128], F32)
        bONES = Buf("ONES", track=False)
        bMASK = Buf("MASK", track=False)
        bIDN = Buf("IDN", track=False)
        bIDF = Buf("IDF")
        V_TM = sb("V_TM", [128, 4, N], BF16)
        bV = [Buf(f"V{b}") for b in range(4)]
        KD_TM = sb("KD_TM", [128, 2, N], BF16)
        bKD = [Buf(f"KD{c}") for c in range(2)]
        QE = sb("QE", [128, 2, N], BF16)
        bQE = [Buf(f"QE{c}") for c in range(2)]
        KEZ = sb("KEZ", [128, 4, N], BF16)
        bKE = [Buf(f"KE{c}") for c in range(2)]
        SILU = sb("SILU", [128, 4, N], BF16)
        bSILU = [Buf(f"SILU{h}") for h in range(4)]
        GY = sb("GY", [128, 2, N], BF16)
        bGY = [Buf(f"GY{c}") for c in range(2)]
        U = sb("U", [128, 2, 528], F32)
        bU = [Buf(f"U{c}") for c in range(2)]
        XR = sb("XR", [128, 2, 516], F32)
        bXR = [Buf(f"XR{c}") for c in range(2)]
        SCM = sb("SCM", [128, 2, N], BF16)
        scring = Ring("SCM", 2, lambda i: SCM[:, i, :])
        Dd = sb("Dd", [128, 2, 4], F32)
        bDd = [Buf(f"Dd{c}") for c in range(2)]
        Sf = sb("Sf", [128, DEPTH, 2, 128], F32)
        Sb = sb("Sb", [128, DEPTH, 4, 128], BF16)
        bSf = [[Buf(f"Sf{l}_{hp}") for hp in range(2)] for l in range(DEPTH)]
        bSb = [[Buf(f"Sb{l}_{hp}") for hp in range(2)] for l in range(DEPTH)]
        HL = sb("HL", [128, DEPTH, 2], F32)
        bHL = [[Buf(f"HL{l}_{c}") for c in range(2)] for l in range(DEPTH)]
        UH = sb("UH", [128, DEPTH, 2, 16], F32)
        bUH = [[Buf(f"UH{l}_{c}") for c in range(2)] for l in range(DEPTH)]
        XH = sb("XH", [128, DEPTH, 2, 4], F32)
        bXH = [[Buf(f"XH{l}_{c}") for c in range(2)] for l in range(DEPTH)]
        FH = sb("FH", [128, DEPTH, 48, 2], F32)
        bFH = [Buf(f"FH{l}") for l in range(DEPTH)]
        CORR = sb("CORR", [128, 48, 2], F32)
        bCORR = Buf("CORR")
        CTMP = sb("CTMP", [128, 48], F32)
        bCTMP = Buf("CTMP")
        NFM, NFF, NBM, NBF, NWM, NWF = 10, 5, 5, 4, 2, 3
        FRM = sb("FRM", [128, NFM, 528], F32)
        FRF = sb("FRF", [128, NFF, N], F32)
        BRM = sb("BRM", [128, NBM, N], BF16)
        BRF = sb("BRF", [128, NBF, N], BF16)
        WRM = sb("WRM", [128, NWM, 4096], BF16)
        WRF = sb("WRF", [128, NWF, 4096], BF16)
        PSB = [psum(f"PS{i}", [128, N], F32) for i in range(7)]
        PST = psum("PST", [128, 1024], BF16)
        bPST = Buf("PST")

        CM, CF = Cx(), Cx()
        CM.H, CM.bH = MIX, bMIX
        CM.fring = Ring("FM", NFM, lambda i: FRM[:, i, :])
        CM.bring = Ring("BM", NBM, lambda i: BRM[:, i, :])
        CM.pring = Ring("PM", 4, lambda i: PSB[i])
        CM.wring = Ring("WM", NWM, lambda i: i)
        CM.WR, CM.wname = WRM, "wm"
        CF.H, CF.bH = HF, bHF
        CF.fring = Ring("FF", NFF, lambda i: FRF[:, i, :])
        CF.bring = Ring("BF", NBF, lambda i: BRF[:, i, :])
        CF.pring = Ring("PF", 3, lambda i: PSB[4 + i])
        CF.wring = Ring("WF", NWF, lambda i: i)
        CF.WR, CF.wname = WRF, "wf"

        def ACT(out, in_, func, reads, writes, **kw):
            S.act(lambda e: e.activation(out=out, in_=in_, func=func, **kw), reads, writes)

        def MM(out, lhsT, rhs, start, stop, reads, writes):
            S.pe(lambda e: e.matmul(out, lhsT=lhsT, rhs=rhs, start=start, stop=stop), reads, writes)

        def STT(out, in0, scalar, in1, op0, op1, reads, writes):
            S.dve(lambda e: e.scalar_tensor_tensor(out=out, in0=in0, scalar=scalar, in1=in1, op0=op0, op1=op1),
                  reads, writes)

        def TT(eng, out, in0, in1, op, reads, writes):
            getattr(S, eng)(lambda e: e.tensor_tensor(out=out, in0=in0, in1=in1, op=op), reads, writes)

        def TS(eng, out, in0, s1, s2, op0, op1, reads, writes):
            if s2 is None:
                getattr(S, eng)(lambda e: e.tensor_scalar(out=out, in0=in0, scalar1=s1, scalar2=None, op0=op0),
                                reads, writes)
            else:
                getattr(S, eng)(lambda e: e.tensor_scalar(out=out, in0=in0, scalar1=s1, scalar2=s2, op0=op0, op1=op1),
                                reads, writes)

        def CP(eng, out, in_, reads, writes):
            if eng == "act":
                S.act(lambda e: e.activation(out=out, in_=in_, func=AF.Copy), reads, writes)
            else:
                getattr(S, eng)(lambda e: e.tensor_copy(out=out, in_=in_), reads, writes)

        def pcol(l, off, n=1):
            base = l * P_LAYER + off
            return PK[:, base:base + n]

        S.dma("sp", lambda e: e.dma_start(out=PK[:, :], in_=pk[:, :]), "pk", writes=[bPK])
        S.dma("sp", lambda e: e.dma_start(out=CN[:, :], in_=cn[:, :]), "cn", writes=[bCN])

        def load_x(t):
            ts_ = slice(t * N, (t + 1) * N)
            for k in range(8):
                S.dma("sp", (lambda k: lambda e: e.dma_start(
                    out=XTs[t % 2][:, k, :], in_=xT[k * 128:(k + 1) * 128, ts_]))(k),
                    f"xl{t % 2}_{k}", writes=[bXs[t % 2][k]])

        load_x(0)
        load_x(1)
        for l in range(DEPTH):
            S.dma("pool", (lambda l: lambda e: e.dma_start(out=SM[:, l, :], in_=smat[l, :, :]))(l), "sm",
                  writes=[bSM], nodeps=True)
        bW16 = [[Buf(f"W16A_{l}", track=False), Buf(f"W16B_{l}", track=False)] for l in range(DEPTH)]
        for l in range(DEPTH):
            for s in range(NSLAB):
                o, wd = SLAB_OFF[s], SLAB_W[s]
                bb = 2048 if wd == 4096 else 1536
                grp = 0 if s < S_UP else 1
                S.dma("pool", (lambda l, o, wd, bb: lambda e: e.dma_start(
                    out=w16[l, :, o:o + wd].rearrange("p (a b) -> p a b", b=bb),
                    in_=w32[l, :, o:o + wd].rearrange("p (a b) -> p a b", b=bb)))(l, o, wd, bb),
                    f"cv{l}_{grp}", writes=[bW16[l][grp]], nodeps=True)
        S.dve(lambda e: e.memset(ONES[:, :], 1.0), writes=[bONES])
        S.dve(lambda e: e.tensor_copy(out=MASKB[:, :], in_=CN[:, C_MASK:C_MASK + 128]), reads=[bCN], writes=[bMASK])
        S.dve(lambda e: e.memset(IDF[:, :], 1.0), writes=[bIDF])
        S.pool(lambda e: e.affine_select(out=IDF[:, :], in_=IDF[:, :], pattern=[[1, 128]], base=0,
                                         channel_multiplier=-1, compare_op=ALU.is_equal, fill=0.0),
               reads=[bIDF], writes=[bIDF])
        S.pool(lambda e: e.tensor_copy(out=IDN[:, :], in_=IDF[:, :]), reads=[bIDF], writes=[bIDN])
        S.pool(lambda e: e.memset(KEZ[:, :, :], 0.0), writes=bKE)
        for (tl, bl) in ((Sf, bSf), (Sb, bSb)):
            S.pool((lambda tl: lambda e: e.memset(tl[:, :, :, :], 0.0))(tl), writes=[b for r in bl for b in r])
        S.pool(lambda e: e.memset(HL[:, :, :], 0.0), writes=[b for r in bHL for b in r])
        S.pool(lambda e: e.memset(UH[:, :, :, :], 0.0), writes=[b for r in bUH for b in r])
        S.pool(lambda e: e.memset(XH[:, :, :, :], 0.0), writes=[b for r in bXH for b in r])
        S.pool(lambda e: e.memset(FH[:, :, :, :], 0.0), writes=bFH)
        for l in range(DEPTH):
            TS("dve", DPK[:, l, 0:2], pcol(l, P_BG, 2), -1.0, None, ALU.mult, None, [bPK], [bDPK])
            fb, ft = CM.fring.get()
            ACT(ft[:, 0:2], pcol(l, P_LAM, 2), AF.Exp, [bPK], [fb], scale=-1.0)
            ACT(ft[:, 2:4], ft[:, 0:2], AF.Ln, [fb], [fb], bias=1.0)
            TS("dve", DPK[:, l, 2:4], ft[:, 2:4], -8.0, None, ALU.mult, None, [fb], [bDPK])

        def load_slab(cx, l, s):
            b, slot = cx.wring.get()
            o, wd = SLAB_OFF[s], SLAB_W[s]
            grp = 0 if s < S_UP else 1
            WRt = cx.WR
            S.dma("sp", lambda e: e.dma_start(out=WRt[:, slot, 0:wd], in_=w16[l, :, o:o + wd]),
                  f"{cx.wname}{slot}", reads=[bW16[l][grp]], writes=[b])
            return b, slot

        def rmsnorm(cx, X, bX):
            pb, pt = cx.pring.get()
            for k in range(8):
                qb, qt = cx.bring.get()
                ACT(qt, X[:, k, :], AF.Square, [bX[k]], [qb])
                MM(pt[:, :], ONES[:, :], qt, k == 0, k == 7, [bONES, qb], [pb])
            lb, lt = cx.fring.get()
            ACT(lt[:, 0:N], pt[:, :], AF.Ln, [pb], [lb], scale=1.0 / D, bias=EPS)
            rb, rt = cx.fring.get()
            ACT(rt[:, 0:N], lt[:, 0:N], AF.Exp, [lb], [rb], scale=-0.5)
            return rb, rt

        def norm_to_H(cx, X, bX, gcol_base):
            rb, rt = rmsnorm(cx, X, bX)
            for k in range(8):
                STT(cx.H[:, k, :], X[:, k, :], PK[:, gcol_base + k:gcol_base + k + 1], rt[:, 0:N],
                    ALU.mult, ALU.mult, [bX[k], bPK, rb], [cx.bH[k]])

        def norm_gen(cx, X, bX, gcol_base, ph):
            pb, pt = cx.pring.get()
            for k in range(8):
                qb, qt = cx.bring.get()
                ACT(qt, X[:, k, :], AF.Square, [bX[k]], [qb])
                MM(pt[:, :], ONES[:, :], qt, k == 0, k == 7, [bONES, qb], [pb])
                if k == 3:
                    yield ph
            yield ph
            lb, lt = cx.fring.get()
            ACT(lt[:, 0:N], pt[:, :], AF.Ln, [pb], [lb], scale=1.0 / D, bias=EPS)
            ACT(lt[:, 0:N], lt[:, 0:N], AF.Exp, [lb], [lb], scale=-0.5)
            yield ph
            for k in range(8):
                STT(cx.H[:, k, :], X[:, k, :], PK[:, gcol_base + k:gcol_base + k + 1], lt[:, 0:N],
                    ALU.mult, ALU.mult, [bX[k], bPK, lb], [cx.bH[k]])
                if k % 2 == 1:
                    yield ph

        def fm_group(cx, slot, wb, col0, M=128):
            pb, pt = cx.pring.get()
            for k in range(8):
                MM(pt[0:M, :], cx.WR[:, slot, k * 512 + col0:k * 512 + col0 + M], cx.H[:, k, :], k == 0, k == 7,
                   [wb, cx.bH[k]], [pb])
            return pb, pt

        pref = {}

        def mixer_gen(t, l, nxt=None):
            cx = CM
            X, bX = XTs[t % 2], bXs[t % 2]
            H, bH = cx.H, cx.bH
            fring, bring, pring = cx.fring, cx.bring, cx.pring
            first_tile = (t == 0)
            yield from norm_gen(cx, X, bX, l * P_LAYER + P_G1, 1)
            if ("M", t, l) in pref:
                wb, slot = pref.pop(("M", t, l))
            else:
                wb, slot = load_slab(cx, l, S_IN + 0)
            pb, pt = fm_group(cx, slot, wb, 256, M=16)
            gb_, GLOW = bring.get()
            ACT(GLOW[0:16, :], pt[0:16, :], AF.Copy, [pb], [gb_])
            yield 1
            cs = []
            for c in range(2):
                pb, pt = pring.get()
                MM(pt[:, :], SM[0:16, l, c * 128:(c + 1) * 128], GLOW[0:16, :], True, True, [bSM, gb_], [pb])
                eb_, et_ = fring.get()
                ACT(et_[:, 0:N], pt[:, :], AF.Exp, [pb, bDPK], [eb_], scale=-1.0, bias=DPK[:, l, c:c + 1])
                sb_, st_ = fring.get()
                ACT(st_[:, 0:N], et_[:, 0:N], AF.Ln, [eb_], [sb_], bias=1.0)
                cb_, ct_ = fring.get()
                S.dve((lambda ct_, st_: lambda e: e.tensor_tensor_scan(
                    out=ct_[:, 0:N], data0=CN[:, C_RESET:C_RESET + N], data1=st_[:, 0:N], initial=0.0,
                    op0=ALU.mult, op1=ALU.add))(ct_, st_), [sb_, bCN], [cb_])
                cs.append((cb_, ct_))
                yield 1
            ebs, enbs = [], []
            for c in range(2):
                cb_, ct_ = cs[c]
                b1, t1 = fring.get()
                ACT(t1[:, 0:N], ct_[:, 0:N], AF.Exp, [cb_], [b1], scale=-1.0 / 16)
                b2, t2 = fring.get()
                ACT(t2[:, 0:N], ct_[:, 0:N], AF.Exp, [cb_], [b2], scale=1.0 / 16)
                ACT(Dd[:, c, :], ct_[:, 127:N:128], AF.Exp, [cb_], [bDd[c]], scale=-1.0 / 16)
                ebs.append((b1, t1))
                enbs.append((b2, t2))
            for c in range(2):
                pb, pt = fm_group(cx, slot, wb, c * 128)
                ACT(GY[:, c, :], pt[:, :], AF.Gelu_apprx_tanh, [pb], [bGY[c]])
                yield 1
            wb, slot = load_slab(cx, l, S_IN + 1)
            for c in range(2):
                pb, pt = fm_group(cx, slot, wb, c * 128)
                STT(QE[:, c, :], pt[:, :], 0.125, ebs[c][1][:, 0:N], ALU.mult, ALU.mult, [pb, ebs[c][0]], [bQE[c]])
                yield 1
            for c in range(2):
                pb, pt = fm_group(cx, slot, wb, 256 + c * 128)
                ent = enbs[c][1]
                for a in range(2):
                    ps_ = slice(a * 64, (a + 1) * 64)
                    TT("dve", KEZ[ps_, 2 * c + a, :], pt[ps_, :], ent[ps_, 0:N], ALU.mult, [pb, enbs[c][0]], [bKE[c]])
                db_, dt_ = fring.get()
                for blk in range(4):
                    TS("dve", dt_[:, blk * 128:(blk + 1) * 128], ent[:, blk * 128:(blk + 1) * 128],
                       Dd[:, c, blk:blk + 1], None, ALU.mult, None, [enbs[c][0], bDd[c]], [db_])
                kb_, KDT = bring.get()
                TT("dve", KDT, pt[:, :], dt_[:, 0:N], ALU.mult, [pb, db_], [kb_])
                yield 1
                for blk in range(4):
                    S.pe((lambda KDT, blk: lambda e: e.transpose(
                        PST[:, blk * 128:(blk + 1) * 128], KDT[:, blk * 128:(blk + 1) * 128], IDN[:, :]))(KDT, blk),
                        [kb_, bIDN], [bPST])
                CP("act", KD_TM[:, c, :], PST[:, 0:N], [bPST], [bKD[c]])
                yield 1
            wb, slot = load_slab(cx, l, S_IN + 2)
            for blk in range(4):
                pb, pt = pring.get()
                for k in range(8):
                    MM(pt[:, :], H[:, k, blk * 128:(blk + 1) * 128], cx.WR[:, slot, k * 512:(k + 1) * 512],
                       k == 0, k == 7, [wb, bH[k]], [pb])
                CP("act", V_TM[:, blk, :], pt[:, :], [pb], [bV[blk]])
                yield 1
            wb, slot = load_slab(cx, l, S_IN + 3)
            for h in range(4):
                pb, pt = fm_group(cx, slot, wb, h * 128)
                fb, ft = fring.get()
                ACT(ft[:, 0:N], pt[:, :], AF.Silu, [pb], [fb])
                TS("dve", SILU[:, h, :], ft[:, 0:N], pcol(l, P_GN + h), None, ALU.mult, None, [fb, bPK], [bSILU[h]])
                yield 1
            wb, slot = load_slab(cx, l, S_IN + 4)
            for c in range(2):
                CP("pool", U[:, c, 0:16], UH[:, l, c, :], [bUH[l][c]], [bU[c]])
                pb, pt = fm_group(cx, slot, wb, c * 128)
                CP("act", U[:, c, 16:528], pt[:, :], [pb], [bU[c]])
                CP("pool", UH[:, l, c, :], U[:, c, 512:528], [bU[c]], [bUH[l][c]])
                yield 1
            for c in range(2):
                CP("pool", XR[:, c, 0:3], XH[:, l, c, 0:3], [bXH[l][c]], [bXR[c]])
                pb, pt = fm_group(cx, slot, wb, 256 + c * 128)
                CP("act", XR[:, c, 3:515], pt[:, :], [pb], [bXR[c]])
                CP("pool", XH[:, l, c, 0:3], XR[:, c, 512:515], [bXR[c]], [bXH[l][c]])
                yield 1

            psrc = []
            for c in range(2):
                u = U[:, c, :]
                b2, t2 = fring.get()
                TT("pool", t2[:, 1:528], u[:, 1:528], u[:, 0:527], ALU.add, [bU[c]], [b2])
                b4, t4 = fring.get()
                TT("pool", t4[:, 3:528], t2[:, 3:528], t2[:, 1:526], ALU.add, [b2], [b4])
                if c == 0:
                    psrc.append(((b2, t2, 2, 0), (b4, t4, 4, 1)))
                else:
                    b8, t8 = fring.get()
                    TT("pool", t8[:, 7:528], t4[:, 7:528], t4[:, 3:524], ALU.add, [b4], [b8])
                    b16, t16 = fring.get()
                    TT("pool", t16[:, 15:528], t8[:, 15:528], t8[:, 7:520], ALU.add, [b8], [b16])
                    psrc.append(((b8, t8, 8, 2), (b16, t16, 16, 3)))
            xcs = []
            for c in range(2):
                xb_, xc = fring.get()
                TS("pool", xc[:, 0:N], XR[:, c, 3:515], pcol(l, P_LCW + c * 4 + 3), pcol(l, P_LCB + c),
                   ALU.mult, ALU.add, [bXR[c], bPK], [xb_])
                xcs.append((xb_, xc))
            yield 2
            yield 2
            yield 2
            dpls = []
            if first_tile:
                fb, ft = fring.get()
            for c in range(2):
                u = U[:, c, :]
                dpb, DPLc = bring.get()
                for a, (sbuf_, stile, win, wi) in enumerate(psrc[c]):
                    ps_ = slice(a * 64, (a + 1) * 64)
                    STT(DPLc[ps_, :], stile[ps_, 16:528], 1.0 / win, u[ps_, 16:528], ALU.mult, ALU.subtract,
                        [sbuf_, bU[c]], [dpb])
                    if first_tile:
                        fcol = slice(c * 16, (c + 1) * 16)
                        TT("dve", ft[ps_, fcol], stile[ps_, 16:32], CN[ps_, C_INV + wi * 16:C_INV + (wi + 1) * 16],
                           ALU.mult, [sbuf_, bCN], [fb])
                        TT("dve", DPLc[ps_, 0:16], ft[ps_, fcol], u[ps_, 16:32], ALU.subtract,
                           [fb, bU[c], dpb], [dpb])
                dpls.append((dpb, DPLc))
            for c in range(2):
                xb_, xc = xcs[c]
                for k in range(3):
                    STT(xc[:, 0:N], XR[:, c, k:k + N], pcol(l, P_LCW + c * 4 + k), xc[:, 0:N], ALU.mult, ALU.add,
                        [bXR[c], bPK, xb_], [xb_])
            yield 2
            yield 2
            cbs = []
            for c in range(2):
                cbb, cbt = bring.get()
                CP("pool", cbt, xcs[c][1][:, 0:N], [xcs[c][0]], [cbb])
                cbs.append((cbb, cbt))
            pps = []
            for c in range(2):
                pb, pt = pring.get()
                MM(pt[:, :], SM[:, l, 256 + c * 128:256 + (c + 1) * 128], dpls[c][1], True, True, [bSM, dpls[c][0]], [pb])
                pps.append((pb, pt))
            yield 2
            yield 2
            for c in range(2):
                pb, pt = pps[c]
                ACT(MIX[:, 4 + c, :], pt[:, :], AF.Identity, [pb, bPK], [bMIX[4 + c]], scale=pcol(l, P_PSC + c))
            lr = []
            for c in range(2):
                cbb, cbt = cbs[c]
                pa, pat = pring.get()
                MM(pat[:, :], SM[:, l, 512 + c * 128:512 + (c + 1) * 128], cbt, True, True, [bSM, cbb], [pa])
                pi, pit = pring.get()
                MM(pit[:, :], SM[:, l, 768 + c * 128:768 + (c + 1) * 128], cbt, True, True, [bSM, cbb], [pi])
                yield 2
                rb_, rt_ = fring.get()
                ACT(rt_[:, 0:N], pat[:, :], AF.Sigmoid, [pa, bPK], [rb_], bias=pcol(l, P_LBA + c))
                ib_, it_ = fring.get()
                ACT(it_[:, 0:N], pit[:, :], AF.Sigmoid, [pi, bPK], [ib_], bias=pcol(l, P_LBX + c))
                lr.append((rb_, rt_, ib_, it_))
            yield 2
            for c in range(2):
                rb_, rt_, ib_, it_ = lr[c]
                ACT(rt_[:, 0:N], rt_[:, 0:N], AF.Exp, [rb_, bDPK], [rb_], scale=DPK[:, l, 2 + c:3 + c])
                TT("pool", it_[:, 0:N], it_[:, 0:N], xcs[c][1][:, 0:N], ALU.mult, [ib_, xcs[c][0]], [ib_])
            yield 2
            qs = []
            for c in range(2):
                rb_, rt_, ib_, it_ = lr[c]
                a2b, a2t = fring.get()
                TT("dve", a2t[:, 0:N], rt_[:, 0:N], rt_[:, 0:N], ALU.mult, [rb_], [a2b])
                qs.append((a2b, a2t))
            yield 2
            for c in range(2):
                a2b, a2t = qs[c]
                ACT(a2t[:, 0:N], a2t[:, 0:N], AF.Sqrt, [a2b], [a2b], scale=-1.0, bias=1.0)
            yield 2
            yield 2
            for c in range(2):
                rb_, rt_, ib_, it_ = lr[c]
                a2b, a2t = qs[c]
                xb_, xc = xcs[c]
                TT("dve", a2t[:, 0:N], a2t[:, 0:N], it_[:, 0:N], ALU.mult, [a2b, ib_], [a2b])
                S.dve((lambda xc, rt_, a2t, c: lambda e: e.tensor_tensor_scan(
                    out=xc[:, 0:N], data0=rt_[:, 0:N], data1=a2t[:, 0:N], initial=HL[:, l, c:c + 1],
                    op0=ALU.mult, op1=ALU.add))(xc, rt_, a2t, c), [rb_, a2b, bHL[l][c]], [xb_])
                CP("dve", HL[:, l, c:c + 1], xc[:, N - 1:N], [xb_], [bHL[l][c]])
                TT("dve", MIX[:, 6 + c, :], xc[:, 0:N], GY[:, c, :], ALU.mult, [xb_, bGY[c]], [bMIX[6 + c]])
            yield 2

            g_sc, g_kv, g_st, g_ot, g_q, g_pre, g_ss, g_rt = {}, {}, {}, {}, {}, {}, {}, {}

            def gla_stage(s, blk):
                bs = slice(blk * 128, (blk + 1) * 128)
                if s == 0:
                    pb, pt = pring.get()
                    for h in range(4):
                        hp, a = divmod(h, 2)
                        MM(pt[:, h * 128:(h + 1) * 128], KEZ[:, h, bs], QE[:, hp, bs], True, True,
                           [bKE[hp], bQE[hp]], [pb])
                    g_sc[blk] = (pb, pt)
                    kb, kt = pring.get()
                    for hp in range(2):
                        MM(kt[:, hp * 256:(hp + 1) * 256], KD_TM[:, hp, bs], V_TM[:, blk, hp * 256:(hp + 1) * 256],
                           True, True, [bKD[hp], bV[blk]], [kb])
                    g_kv[blk] = (kb, kt)
                elif s == 1:
                    pb, pt = g_sc[blk]
                    sb_, st_ = scring.get()
                    TT("dve", st_.rearrange("p (h n) -> p h n", h=4), pt[:, :].rearrange("p (h n) -> p h n", h=4),
                       MASKB[:, :].unsqueeze(1).to_broadcast([128, 4, 128]), ALU.mult, [pb, bMASK], [sb_])
                    g_st[blk] = (sb_, st_)
                    kb, kt = g_kv[blk]
                    for hp in range(2):
                        for a in range(2):
                            ps_ = slice(a * 64, (a + 1) * 64)
                            STT(Sf[ps_, l, hp, :], Sf[ps_, l, hp, :], Dd[ps_, hp, blk:blk + 1],
                                kt[ps_, hp * 256 + a * 128:hp * 256 + (a + 1) * 128], ALU.mult, ALU.add,
                                [bSf[l][hp], bDd[hp], kb], [bSf[l][hp]])
                elif s == 2:
                    sb_, st_ = g_st[blk]
                    ob, ot = pring.get()
                    for h in range(4):
                        hp, a = divmod(h, 2)
                        MM(ot[:, h * 128:(h + 1) * 128], V_TM[:, blk, h * 128:(h + 1) * 128],
                           st_[:, h * 128:(h + 1) * 128], True, False, [bV[blk], sb_], [ob])
                        MM(ot[:, h * 128:(h + 1) * 128], Sb[:, l, h, :], QE[:, hp, bs], False, True,
                           [bSb[l][hp], bQE[hp]], [ob])
                    g_ot[blk] = (ob, ot)
                    for hp in range(2):
                        for a in range(2):
                            ps_ = slice(a * 64, (a + 1) * 64)
                            CP("act", Sb[ps_, l, 2 * hp + a, :], Sf[ps_, l, hp, :], [bSf[l][hp]], [bSb[l][hp]])
                elif s == 3:
                    ob, ot = g_ot[blk]
                    qb, qt = bring.get()
                    ACT(qt, ot[:, :], AF.Square, [ob], [qb])
                    g_q[blk] = (qb, qt)
                    tb, tt = fring.get()
                    TT("dve", tt[:, 0:N].rearrange("p (h n) -> p h n", h=4),
                       ot[:, :].rearrange("p (h n) -> p h n", h=4), SILU[:, :, bs], ALU.mult, [ob] + bSILU, [tb])
                    g_pre[blk] = (tb, tt)
                elif s == 4:
                    qb, qt = g_q[blk]
                    nb_, nt_ = pring.get()
                    MM(nt_[:, :], ONES[:, :], qt, True, True, [bONES, qb], [nb_])
                    g_ss[blk] = (nb_, nt_)
                elif s == 5:
                    nb_, nt_ = g_ss[blk]
                    lb, lt = fring.get()
                    ACT(lt[:, 0:N], nt_[:, :], AF.Ln, [nb_], [lb], scale=1.0 / 128, bias=EPS)
                    ACT(lt[:, 0:N], lt[:, 0:N], AF.Exp, [lb], [lb], scale=-0.5)
                    g_rt[blk] = (lb, lt)
                elif s == 6:
                    tb, tt = g_pre[blk]
                    lb, lt = g_rt[blk]
                    TT("dve", MIX[:, 0:4, bs], tt[:, 0:N].rearrange("p (h n) -> p h n", h=4),
                       lt[:, 0:N].rearrange("p (h n) -> p h n", h=4), ALU.mult, [tb, lb], bMIX[0:4])

            for slot in range(2 * 3 + 7):
                for s in range(6, -1, -1):
                    if (slot - s) % 2 == 0 and 0 <= (slot - s) // 2 < 4:
                        gla_stage(s, (slot - s) // 2)
                yield 2

            slabs = [load_slab(cx, l, S_OUT + i) for i in range(2)]
            for n in range(8):
                if n == 4 and nxt is not None:
                    pref[("M",) + nxt] = load_slab(cx, nxt[1], S_IN + 0)
                wb, slot = slabs[n // 4]
                col0 = (n % 4) * 128
                pb, pt = pring.get()
                for k in range(8):
                    MM(pt[:, :], cx.WR[:, slot, k * 512 + col0:k * 512 + col0 + 128], MIX[:, k, :], k == 0, k == 7,
                       [wb, bMIX[k]], [pb])
                TT("dve", X[:, n, :], X[:, n, :], pt[:, :], ALU.add, [bX[n], pb], [bX[n]])
                yield 3

        def ffn_pre(t, l):
            cx = CF
            X, bX = XTs[t % 2], bXs[t % 2]
            yield from norm_gen(cx, X, bX, l * P_LAYER + P_G2, 4)
            fcw = PK[:, l * P_LAYER + P_FCW:l * P_LAYER + P_FCW + 144].rearrange("p (g k) -> p g k", k=3)
            TT("dve", CORR[:, :, 1], FH[:, l, :, 1], fcw[:, :, 0], ALU.mult, [bFH[l], bPK], [bCORR])
            TT("dve", CORR[:, :, 0], FH[:, l, :, 1], fcw[:, :, 1], ALU.mult, [bFH[l], bPK], [bCORR])
            TT("dve", CTMP[:, :], FH[:, l, :, 0], fcw[:, :, 0], ALU.mult, [bFH[l], bPK], [bCTMP])
            TT("dve", CORR[:, :, 0], CORR[:, :, 0], CTMP[:, :], ALU.add, [bCORR, bCTMP], [bCORR])
            yield 4

        def ffn_gen(t, l, nxt=None):
            cx = CF
            X, bX = XTs[t % 2], bXs[t % 2]
            fring, bring, pring = cx.fring, cx.bring, cx.pring
            if not EARLY_PRE:
                yield from ffn_pre(t, l)

            def conv_group(slot, wb, col0, g):
                pb, pt = fm_group(cx, slot, wb, col0)
                ab_, at_ = fring.get()
                base = l * P_LAYER + P_FCW + g * 3
                ACT(at_[:, 0:N], pt[:, :], AF.Identity, [pb, bPK], [ab_], scale=PK[:, base + 2:base + 3],
                    bias=pcol(l, P_FCB + g))
                STT(at_[:, 1:N], pt[:, 0:N - 1], PK[:, base + 1:base + 2], at_[:, 1:N], ALU.mult, ALU.add,
                    [pb, bPK, ab_], [ab_])
                STT(at_[:, 2:N], pt[:, 0:N - 2], PK[:, base:base + 1], at_[:, 2:N], ALU.mult, ALU.add,
                    [pb, bPK, ab_], [ab_])
                TT("pool", at_[:, 0:2], at_[:, 0:2], CORR[:, g, :], ALU.add, [ab_, bCORR], [ab_])
                CP("act", FH[:, l, g, :], pt[:, N - 2:N], [pb], [bFH[l]])
                return ab_, at_

            def gating(fch, gb, gt, vb, vt):
                ggb, ggt = bring.get()
                ACT(ggt, gt[:, 0:N], AF.Gelu_apprx_tanh, [gb], [ggb])
                TT("pool", A24[:, fch, :], ggt, vt[:, 0:N], ALU.mult, [ggb, vb], [bA24[fch]])

            pend = None
            for i in range(6):
                if i == 0 and ("F", t, l) in pref:
                    (wg, sg), (wv, sv) = pref.pop(("F", t, l))
                else:
                    wg, sg = load_slab(cx, l, S_UP + 2 * i)
                    wv, sv = load_slab(cx, l, S_UP + 2 * i + 1)
                for j in range(4):
                    fch = 4 * i + j
                    gb, gt = conv_group(sg, wg, j * 128, fch)
                    yield 8
                    vb, vt = conv_group(sv, wv, j * 128, 24 + fch)
                    if pend is not None:
                        gating(*pend)
                    pend = (fch, gb, gt, vb, vt)
                    yield 8
            gating(*pend)
            for n in range(8):
                wb, slot = load_slab(cx, l, S_DN + n)
                if n == 7 and nxt is not None:
                    pref[("F",) + nxt] = [load_slab(cx, nxt[1], S_UP + 0), load_slab(cx, nxt[1], S_UP + 1)]
                pb, pt = pring.get()
                for j in range(24):
                    MM(pt[:, :], cx.WR[:, slot, j * 128:(j + 1) * 128], A24[:, j, :], j == 0, j == 23,
                       [wb, bA24[j]], [pb])
                    if j % 8 == 7:
                        yield 8
                TT("dve", X[:, n, :], X[:, n, :], pt[:, :], ALU.add, [bX[n], pb], [bX[n]])
            if l == DEPTH - 1:
                rb, rt = rmsnorm(cx, X, bX)
                gb = DEPTH * P_LAYER
                for k in range(8):
                    STT(X[:, k, :], X[:, k, :], PK[:, gb + k:gb + k + 1], rt[:, 0:N], ALU.mult, ALU.mult,
                        [bX[k], bPK, rb], [bX[k]])
                ts_ = slice(t * N, (t + 1) * N)
                for k in range(8):
                    S.dma("act", (lambda k: lambda e: e.dma_start(
                        out=outT[k * 128:(k + 1) * 128, ts_], in_=X[:, k, :]))(k), f"st{t % 2}_{k}", reads=[bX[k]])
                if t + 2 < NT:
                    load_x(t + 2)
                yield 8

        TL = [(t, l) for pair in range(NT // 2) for l in range(DEPTH) for t in (2 * pair, 2 * pair + 1)]
        def mside(t, l, nxt=None):
            yield from mixer_gen(t, l, nxt)
            if EARLY_PRE:
                yield from ffn_pre(t, l)

        S.dry = True
        cnt = {1: 0, 2: 0, 3: 0, 4: 0}
        for ph in mside(2, 0):
            cnt[ph] += 1
        S.dry = False
        xs = [0]
        for ph in ((1, 2, 3, 4) if EARLY_PRE else (1, 2, 3)):
            xs.append(xs[-1] + cnt[ph])
        ys = M_SCHED_Y if EARLY_PRE else M_SCHED_Y3

        def m_target(i):
            for j in range(1, len(xs)):
                if i <= xs[j]:
                    return ys[j - 1] + (ys[j] - ys[j - 1]) * (i - xs[j - 1]) / float(xs[j] - xs[j - 1])
            return ys[-1]

        for step in range(len(TL) + 1):
            nxt_m = TL[step + 1] if step + 1 < len(TL) else None
            nxt_f = TL[step] if step < len(TL) else None
            gm = mside(*TL[step], nxt=nxt_m) if step < len(TL) else None
            gf = ffn_gen(*TL[step - 1], nxt=nxt_f) if step >= 1 else None
            pm = 0
            pf = 0.0
            while gm is not None or gf is not None:
                run_m = gm is not None and (gf is None or m_target(pm) <= pf / FFN_W)
                if run_m:
                    try:
                        next(gm)
                        pm += 1
                    except StopIteration:
                        gm = None
                else:
                    try:
                        pf += next(gf)
                    except StopIteration:
                        gf = None
        global _SBUF_LEFT
        _SBUF_LEFT = nc.sbuf_bytes_remaining
        S.emit(final_waits=[f"st{p}_{k}" for p in range(2) for k in range(8)])
    return nc


MIXER_W = 54.0
EARLY_PRE = False
M_SCHED_Y = (0.02, 0.36, 0.80, 0.86, 0.95)
M_SCHED_Y3 = (0.0, 0.50, 0.96, 1.0)
FFN_W = 8.0 * (48 + 24)


def _slab_k(Wc):
    return np.ascontiguousarray(Wc.reshape(8, 128, 512).transpose(1, 0, 2)).reshape(128, 4096)


def _fm(v, n):
    return np.ascontiguousarray(v.reshape(n, 128).T)


def prep_host(inp, DEPTH):
    w32 = np.zeros((DEPTH, 128, WCOLS), np.float32)
    smat = np.zeros((DEPTH, 128, 1024), np.float32)
    pk = np.zeros((128, DEPTH * P_LAYER + 8), np.float32)
    for l in range(DEPTH):
        wi = inp["w_in"][l]
        q, k, v, g = wi[:, 0:256], wi[:, 256:512], wi[:, 512:1024], wi[:, 1024:1536]
        glow, pu, lx, ly = wi[:, 1536:1552], wi[:, 1552:1808], wi[:, 1808:2064], wi[:, 2064:2320]
        z = np.zeros((1024, 240), np.float32)
        slabs = [np.concatenate([ly, glow, z], 1), np.concatenate([q, k], 1), v, g, np.concatenate([pu, lx], 1)]
        wo = inp["w_out"][l]
        slabs += [wo[:, 0:512], wo[:, 512:1024]]
        wu = inp["ffn_w_up"][l]
        for i in range(6):
            slabs += [wu[:, 512 * i:512 * (i + 1)], wu[:, 3072 + 512 * i:3072 + 512 * (i + 1)]]
        for s, sl in enumerate(slabs):
            w32[l, :, SLAB_OFF[s]:SLAB_OFF[s + 1]] = _slab_k(sl)
        wd = inp["ffn_w_down"][l]
        for n in range(8):
            blk = wd[:, n * 128:(n + 1) * 128].reshape(24, 128, 128).transpose(1, 0, 2).reshape(128, 3072)
            s = S_DN + n
            w32[l, :, SLAB_OFF[s]:SLAB_OFF[s + 1]] = blk
        smat[l, 0:16, 0:256] = inp["gla_wg2"][l]
        for c in range(2):
            for a in range(2):
                r = slice(a * 64, (a + 1) * 64)
                smat[l, r, 256 + c * 128 + a * 64:256 + c * 128 + (a + 1) * 64] = inp["pool_w"][l, 2 * c + a]
                smat[l, r, 512 + c * 128 + a * 64:512 + c * 128 + (a + 1) * 64] = inp["lru_wa"][l, 2 * c + a]
                smat[l, r, 768 + c * 128 + a * 64:768 + c * 128 + (a + 1) * 64] = inp["lru_wx"][l, 2 * c + a]
        b = l * P_LAYER
        pk[:, b + P_G1:b + P_G1 + 8] = _fm(inp["norm1_g"][l], 8)
        pk[:, b + P_G2:b + P_G2 + 8] = _fm(inp["norm2_g"][l], 8)
        pk[:, b + P_BG:b + P_BG + 2] = _fm(inp["gla_bg"][l], 2)
        pk[:, b + P_GN:b + P_GN + 4] = inp["gla_norm_g"][l].T
        pk[:, b + P_PSC:b + P_PSC + 2] = _fm(inp["pool_scale"][l], 2)
        for c in range(2):
            pk[:, b + P_LCW + c * 4:b + P_LCW + c * 4 + 4] = inp["lru_conv_w"][l][:, c * 128:(c + 1) * 128].T
        pk[:, b + P_LCB:b + P_LCB + 2] = _fm(inp["lru_conv_b"][l], 2)
        pk[:, b + P_LBA:b + P_LBA + 2] = _fm(inp["lru_ba"][l], 2)
        pk[:, b + P_LBX:b + P_LBX + 2] = _fm(inp["lru_bx"][l], 2)
        pk[:, b + P_LAM:b + P_LAM + 2] = _fm(inp["lru_lambda"][l], 2)
        fw = inp["ffn_conv_w"][l]
        pk[:, b + P_FCW:b + P_FCW + 144] = fw.reshape(3, 48, 128).transpose(2, 1, 0).reshape(128, 144)
        pk[:, b + P_FCB:b + P_FCB + 48] = _fm(inp["ffn_conv_b"][l], 48)
    pk[:, DEPTH * P_LAYER:DEPTH * P_LAYER + 8] = _fm(inp["final_g"], 8)
    cn = np.zeros((128, NCN), np.float32)
    j = np.arange(128)[:, None]
    i = np.arange(128)[None, :]
    cn[:, C_MASK:C_MASK + 128] = (j <= i)
    rs = np.ones(512, np.float32)
    rs[::128] = 0.0
    cn[:, C_RESET:C_RESET + 512] = rs[None, :]
    for wi_, win in enumerate((2, 4, 8, 16)):
        cn[:, C_INV + wi_ * 16:C_INV + (wi_ + 1) * 16] = (1.0 / np.minimum(np.arange(1, 17), win))[None, :]
    return w32, smat, pk, cn


_NC_CACHE = {}


def run(inp, S_len, DEPTH, n_cores=8):
    key = (S_len, DEPTH)
    if key not in _NC_CACHE:
        _NC_CACHE[key] = build_nc(S_len, DEPTH)
    nc = _NC_CACHE[key]
    w32, smat, pk, cn = prep_host(inp, DEPTH)
    x = inp["x"]
    in_maps = []
    for b in range(n_cores):
        in_maps.append({"xT": np.ascontiguousarray(x[b].T), "w32": w32, "smat": smat, "pk": pk, "cn": cn})
    res = run_bass_kernel_spmd(nc, in_maps, core_ids=list(range(n_cores)))
    out = np.stack([np.ascontiguousarray(res.results[b]["outT"].T) for b in range(n_cores)], 0)
    return out.astype(np.float32)


def kernel(**inputs):
    inp = {k: np.asarray(v) for k, v in inputs.items()}
    return run(inp, 4096, 4, 8)
```
